# Optimizing a Trainium2 kernel written in Bass

```python
import jax, jax.numpy as jnp
from jax import lax
import numpy as np

D_MODEL = 1024
BATCH = 8
SEQ = 4096
DEPTH = 1

N_MEM = 256
RMS_EPS = 1e-6
LN_EPS = 1e-5

POOL_WINDOWS = (2, 4, 8, 16)
N_POOL_GROUPS = len(POOL_WINDOWS)
POOL_WIDTH = D_MODEL
POOL_GROUP = POOL_WIDTH // N_POOL_GROUPS

SGU_WIDTH = D_MODEL
SGU_CHUNK = 128
SGU_HEADS = 8
SGU_HEAD_DIM = SGU_WIDTH // SGU_HEADS

XA_HEADS = 4
XA_HEAD_DIM = D_MODEL // XA_HEADS
XA_WIDTH = XA_HEADS * XA_HEAD_DIM

N_BRANCHES = 3
GATE_WIDTH = N_BRANCHES * D_MODEL
IN_WIDTH = POOL_WIDTH + 2 * SGU_WIDTH + XA_WIDTH + GATE_WIDTH
SPLIT_POINTS = (POOL_WIDTH,
                POOL_WIDTH + SGU_WIDTH,
                POOL_WIDTH + 2 * SGU_WIDTH,
                POOL_WIDTH + 2 * SGU_WIDTH + XA_WIDTH)

PEER_HEADS = 8
PEER_N_KEYS = 128
PEER_N_EXPERTS = PEER_N_KEYS * PEER_N_KEYS
PEER_QUERY_DIM = 256
PEER_HALF = PEER_QUERY_DIM // 2
PEER_TOPK = 16
PEER_TOKEN_BLOCK = 128

kernel_name = "hybrid_pool_sgu_xattn_peer"


def rmsnorm(x, gain):
    xf = x.astype(jnp.float32)
    y = xf * lax.rsqrt(jnp.mean(xf * xf, axis=-1, keepdims=True) + RMS_EPS)
    return (y * gain.astype(jnp.float32)).astype(x.dtype)


def layernorm(x, gain, bias):
    xf = x.astype(jnp.float32)
    mu = jnp.mean(xf, axis=-1, keepdims=True)
    var = jnp.mean(jnp.square(xf - mu), axis=-1, keepdims=True)
    y = (xf - mu) * lax.rsqrt(var + LN_EPS)
    return (y * gain.astype(jnp.float32) + bias.astype(jnp.float32)).astype(x.dtype)


def causal_multiscale_pool(p, w_pool, pool_scale):
    B, S, _ = p.shape
    pf = p.astype(jnp.float32)
    cs = jnp.cumsum(pf, axis=1)
    t = jnp.arange(S)
    means = []
    for g, w in enumerate(POOL_WINDOWS):
        c = cs[..., g * POOL_GROUP:(g + 1) * POOL_GROUP]
        lagged = jnp.pad(c, ((0, 0), (w, 0), (0, 0)))[:, :S]
        count = jnp.minimum(t + 1, w).astype(jnp.float32)[None, :, None]
        means.append((c - lagged) / count)
    diff = (jnp.concatenate(means, axis=-1) - pf).astype(p.dtype)
    diff = diff.reshape(B, S, N_POOL_GROUPS, POOL_GROUP)
    y = jnp.einsum('bsgc,gcd->bsgd', diff, w_pool).reshape(B, S, POOL_WIDTH)
    return y * pool_scale


def chunked_spatial_gating(u, v, ln_gain, ln_bias, w_s, b_s, w_o):
    B, S, _ = u.shape
    u = jax.nn.gelu(u, approximate=False)
    v = layernorm(jax.nn.gelu(v, approximate=False), ln_gain, ln_bias)
    n_chunks = S // SGU_CHUNK
    vc = v.reshape(B, n_chunks, SGU_CHUNK, SGU_HEADS, SGU_HEAD_DIM)
    causal = jnp.tril(jnp.ones((SGU_CHUNK, SGU_CHUNK), dtype=bool))
    w_masked = jnp.where(causal[None], w_s, jnp.zeros((), w_s.dtype))
    mixed = jnp.einsum('hts,bcshd->bcthd', w_masked, vc) + b_s.T[None, None, :, :, None]
    gated = u * mixed.reshape(B, S, SGU_WIDTH)
    return gated @ w_o


def memory_cross_attention(q, mem_n, w_kv, w_o):
    B, S, _ = q.shape
    M = mem_n.shape[1]
    kv = mem_n @ w_kv
    k, v = jnp.split(kv, 2, axis=-1)
    qh = q.reshape(B, S, XA_HEADS, XA_HEAD_DIM)
    kh = k.reshape(B, M, XA_HEADS, XA_HEAD_DIM)
    vh = v.reshape(B, M, XA_HEADS, XA_HEAD_DIM)
    scores = jnp.einsum('bshd,bmhd->bhsm', qh, kh).astype(jnp.float32) * (XA_HEAD_DIM ** -0.5)
    probs = jax.nn.softmax(scores, axis=-1).astype(vh.dtype)
    o = jnp.einsum('bhsm,bmhd->bshd', probs, vh).reshape(B, S, XA_WIDTH)
    return o @ w_o


def peer_ffn(x, w_q, sub_keys1, sub_keys2, expert_u, expert_v):
    B, S, D = x.shape
    T = B * S
    xt = x.reshape(T, D)
    q = (xt @ w_q).reshape(T, PEER_HEADS, 2, PEER_HALF)
    s1 = jnp.einsum('thc,kc->thk', q[:, :, 0], sub_keys1).astype(jnp.float32)
    s2 = jnp.einsum('thc,kc->thk', q[:, :, 1], sub_keys2).astype(jnp.float32)
    v1, i1 = lax.top_k(s1, PEER_TOPK)
    v2, i2 = lax.top_k(s2, PEER_TOPK)
    cand = (v1[..., :, None] + v2[..., None, :]).reshape(T, PEER_HEADS, PEER_TOPK * PEER_TOPK)
    best, ci = lax.top_k(cand, PEER_TOPK)
    e1 = jnp.take_along_axis(i1, ci // PEER_TOPK, axis=-1)
    e2 = jnp.take_along_axis(i2, ci % PEER_TOPK, axis=-1)
    experts = e1 * PEER_N_KEYS + e2
    gates = jax.nn.softmax(best, axis=-1).astype(x.dtype)
    n_blocks = T // PEER_TOKEN_BLOCK

    def block(args):
        xb, eb, gb = args
        ub = jnp.take(expert_u, eb, axis=0)
        vb = jnp.take(expert_v, eb, axis=0)
        a = jax.nn.gelu(jnp.einsum('td,thkd->thk', xb, ub), approximate=False) * gb
        return jnp.einsum('thk,thkd->td', a, vb)

    y = lax.map(block, (xt.reshape(n_blocks, PEER_TOKEN_BLOCK, D),
                        experts.reshape(n_blocks, PEER_TOKEN_BLOCK, PEER_HEADS, PEER_TOPK),
                        gates.reshape(n_blocks, PEER_TOKEN_BLOCK, PEER_HEADS, PEER_TOPK)))
    return y.reshape(B, S, D)


def setup_inputs(seed: int = 0) -> dict:
    key = jax.random.key(seed)
    ks = jax.random.split(key, 24)
    f32 = jnp.float32
    nrm = lambda k, shape, scale: jax.random.normal(k, shape, f32) * scale
    L, D = DEPTH, D_MODEL
    return {
        "x": nrm(ks[0], (BATCH, SEQ, D), 1.0),
        "mem": nrm(ks[1], (BATCH, N_MEM, D), 1.0),
        "norm1_gain": 1.0 + nrm(ks[2], (L, D), 0.02),
        "w_in": nrm(ks[3], (L, D, IN_WIDTH), D ** -0.5),
        "pool_w": nrm(ks[4], (L, N_POOL_GROUPS, POOL_GROUP, POOL_GROUP), POOL_GROUP ** -0.5),
        "pool_scale": 1.0 + nrm(ks[5], (L, POOL_WIDTH), 0.02),
        "sgu_ln_gain": 1.0 + nrm(ks[6], (L, SGU_WIDTH), 0.02),
        "sgu_ln_bias": nrm(ks[7], (L, SGU_WIDTH), 0.02),
        "sgu_w_s": nrm(ks[8], (L, SGU_HEADS, SGU_CHUNK, SGU_CHUNK), SGU_CHUNK ** -0.5),
        "sgu_b_s": 1.0 + nrm(ks[9], (L, SGU_HEADS, SGU_CHUNK), 0.01),
        "sgu_w_out": nrm(ks[10], (L, SGU_WIDTH, D), SGU_WIDTH ** -0.5),
        "mem_norm_gain": 1.0 + nrm(ks[11], (L, D), 0.02),
        "xa_w_kv": nrm(ks[12], (L, D, 2 * XA_WIDTH), D ** -0.5),
        "xa_w_out": nrm(ks[13], (L, XA_WIDTH, D), XA_WIDTH ** -0.5),
        "w_out": nrm(ks[14], (L, D, D), D ** -0.5),
        "norm2_gain": 1.0 + nrm(ks[15], (L, D), 0.02),
        "peer_w_q": nrm(ks[16], (L, D, PEER_HEADS * PEER_QUERY_DIM), D ** -0.5),
        "peer_keys1": nrm(ks[17], (L, PEER_N_KEYS, PEER_HALF), PEER_HALF ** -0.5),
        "peer_keys2": nrm(ks[18], (L, PEER_N_KEYS, PEER_HALF), PEER_HALF ** -0.5),
        "peer_u": nrm(ks[19], (L, PEER_N_EXPERTS, D), D ** -0.5),
        "peer_v": nrm(ks[20], (L, PEER_N_EXPERTS, D), PEER_HEADS ** -0.5),
        "final_norm_gain": 1.0 + nrm(ks[21], (D,), 0.02),
    }


def reference(x, mem, norm1_gain, w_in, pool_w, pool_scale, sgu_ln_gain, sgu_ln_bias,
              sgu_w_s, sgu_b_s, sgu_w_out, mem_norm_gain, xa_w_kv, xa_w_out, w_out,
              norm2_gain, peer_w_q, peer_keys1, peer_keys2, peer_u, peer_v, final_norm_gain):
    B, S, D = x.shape
    h = x
    for l in range(DEPTH):
        n = rmsnorm(h, norm1_gain[l])
        proj = n @ w_in[l]
        p, u, v, q, g = jnp.split(proj, SPLIT_POINTS, axis=-1)
        gates = jax.nn.sigmoid(g.astype(jnp.float32)).astype(h.dtype).reshape(B, S, N_BRANCHES, D)
        y_pool = causal_multiscale_pool(p, pool_w[l], pool_scale[l])
        y_sgu = chunked_spatial_gating(u, v, sgu_ln_gain[l], sgu_ln_bias[l],
                                       sgu_w_s[l], sgu_b_s[l], sgu_w_out[l])
        y_xa = memory_cross_attention(q, rmsnorm(mem, mem_norm_gain[l]), xa_w_kv[l], xa_w_out[l])
        merged = gates[:, :, 0] * y_pool + gates[:, :, 1] * y_sgu + gates[:, :, 2] * y_xa
        h = h + merged @ w_out[l]
        h = h + peer_ffn(rmsnorm(h, norm2_gain[l]), peer_w_q[l], peer_keys1[l], peer_keys2[l],
                         peer_u[l], peer_v[l])
    return rmsnorm(h, final_norm_gain)
```

```python
import numpy as np
from contextlib import ExitStack
import concourse.bass as bass
import concourse.mybir as mybir
from concourse.bass_utils import run_bass_kernel_spmd

F32 = mybir.dt.float32
BF16 = mybir.dt.bfloat16
U32 = mybir.dt.uint32
AF = mybir.ActivationFunctionType
ALU = mybir.AluOpType
AX = mybir.AxisListType

P = 128
D = 1024
S = 4096
NMEM = 256
INW = 7168
NEXP = 16384
GT = 256
NJ = GT // P
RMS_EPS = 1e-6
LN_EPS = 1e-5
NEG = -1.0e30


class Sched:
    STREAMS = ("pe", "act", "dve", "pool", "sp")

    def __init__(self, nc, es, n_dma_sems=24):
        self.nc = nc
        self.es = es
        self.recs = []
        self.W = {}
        self.R = {}
        self.last = {}
        self.dmas_since_barrier = []
        self.n_dma_sems = n_dma_sems

    def op(self, stream, fn, reads=(), writes=(), dma=False):
        i = len(self.recs)
        deps = {}
        for k in reads:
            for w in self.W.get(k, ()):
                deps[w] = "raw"
        for k in writes:
            rs = self.R.get(k)
            if rs:
                for r in rs:
                    deps.setdefault(r, "war")
                for w in self.W.get(k, ()):
                    deps.setdefault(w, "waw")
                self.W[k] = []
                self.R[k] = []
            else:
                for w in self.W.get(k, ()):
                    deps.setdefault(w, "waw")
        for k in reads:
            self._push(self.R.setdefault(k, []), i, stream, dma)
        for k in writes:
            self._push(self.W.setdefault(k, []), i, stream, dma)
        self.recs.append(dict(stream=stream, fn=fn, dma=dma, deps=deps, need=False, ev=None))
        if fn is not None:
            self.last[stream] = i
        if dma:
            self.dmas_since_barrier.append(i)
        return i

    def _push(self, lst, i, stream, dma):
        if lst and not dma:
            j = lst[-1]
            rj = self.recs[j]
            if rj["stream"] == stream and not rj["dma"]:
                lst[-1] = i
                return
        lst.append(i)

    def barrier(self):
        lasts = dict(self.last)
        dmas = list(self.dmas_since_barrier)
        for s in self.STREAMS:
            deps = {}
            for s2, i in lasts.items():
                if s2 != s or self.recs[i]["dma"]:
                    deps[i] = "raw"
            for i in dmas:
                deps[i] = "raw"
            self.recs.append(dict(stream=s, fn=None, dma=False, deps=deps, need=False, ev=None))
        self.W = {}
        self.R = {}
        self.dmas_since_barrier = []

    def final_wait(self, rec_ids):
        deps = {i: "raw" for i in rec_ids}
        self.recs.append(dict(stream="sp", fn=None, dma=False, deps=deps, need=False, ev=None))

    def lower(self):
        nc = self.nc
        recs = self.recs
        eng = {"pe": nc.tensor, "act": nc.scalar, "dve": nc.vector, "pool": nc.gpsimd, "sp": nc.sync}
        for i, r in enumerate(recs):
            waits = []
            for d, kind in r["deps"].items():
                rd = recs[d]
                if rd["fn"] is None:
                    continue
                if rd["stream"] == r["stream"] and not rd["dma"] and not r["dma"]:
                    if kind != "raw" or r["stream"] == "pe":
                        continue
                waits.append(d)
                rd["need"] = True
            r["waits"] = sorted(waits)
        cnt = {s: 0 for s in self.STREAMS}
        sems = {s: self.es.enter_context(nc.semaphore("sem_" + s)) for s in self.STREAMS}
        semgen = {s: 0 for s in self.STREAMS}
        dsem = [self.es.enter_context(nc.semaphore("dsem%d" % k)) for k in range(self.n_dma_sems)]
        dval = [0] * self.n_dma_sems
        dnext = 0
        seen = {s: {} for s in self.STREAMS}
        n_wait = 0
        for r in recs:
            s = r["stream"]
            e = eng[s]
            sn = seen[s]
            for d in r["waits"]:
                sem, val = recs[d]["ev"]
                if sn.get(id(sem), 0) >= val:
                    continue
                e.wait_ge(sem, val)
                n_wait += 1
                sn[id(sem)] = val
            if r["fn"] is None:
                continue
            if r["dma"]:
                k = dnext
                dnext = (dnext + 1) % self.n_dma_sems
                if dval[k] > 0 and sn.get(id(dsem[k]), 0) < dval[k]:
                    e.wait_ge(dsem[k], dval[k])
                    sn[id(dsem[k])] = dval[k]
                ins = r["fn"](e)
                dval[k] += 16
                ins.then_inc(dsem[k], 16)
                r["ev"] = (dsem[k], dval[k])
            else:
                ins = r["fn"](e)
                if r["need"]:
                    if cnt[s] >= 30000:
                        semgen[s] += 1
                        sems[s] = self.es.enter_context(nc.semaphore("sem_%s_%d" % (s, semgen[s])))
                        cnt[s] = 0
                    cnt[s] += 1
                    ins.then_inc(sems[s], 1)
                    r["ev"] = (sems[s], cnt[s])
        return n_wait


def _const_tables():
    windows = (2, 4, 8, 16)
    s = np.arange(P)[:, None]
    t = np.arange(P)[None, :]
    bands = np.zeros((12, P, P), np.float32)
    for g, w in enumerate(windows):
        cur = ((s <= t) & (s > t - w)).astype(np.float32) / w
        bands[g] = cur - np.eye(P, dtype=np.float32)
        bands[4 + g] = (s > P + t - w).astype(np.float32) / w
        cnt = np.minimum(t + 1, w).astype(np.float32)
        bands[8 + g] = ((s <= t) & (s > t - w)).astype(np.float32) / cnt - np.eye(P, dtype=np.float32)
    consts = {
        "c_ident": np.eye(P, dtype=np.float32),
        "c_bands": np.ascontiguousarray(bands.transpose(1, 0, 2)),
        "c_trilT": (s <= t).astype(np.float32),
        "c_iota": np.tile(np.arange(P, dtype=np.float32)[None, :], (P, 1)),
        "c_iota16": np.tile(np.arange(16, dtype=np.float32)[None, :], (P, 8 * 16)),
    }
    return consts


def build(n_tok=S, debug=False, phases=("A", "B2")):
    nc = bass.Bass("TRN2", target_bir_lowering=False)
    NG = n_tok // GT
    NT = n_tok // P

    def din(name, shape, dt=F32):
        return nc.dram_tensor(name, list(shape), dt, kind="ExternalInput").ap()

    x = din("x", [n_tok, D])
    mem = din("mem", [NMEM, D])
    w_in = din("w_in", [D, INW])
    vecs = din("vecs", [P, 6, 8])
    fg = din("fg", [1, D])
    pool_w = din("pool_w", [4, 256, 256])
    wsT = din("wsT", [P, 8, P])
    bs = din("bs", [1, 8 * P])
    swo = din("swo", [D, D])
    wkv = din("wkv", [D, 2 * D])
    xwo = din("xwo", [D, D])
    wout = din("wout", [D, D])
    wqT = din("wqT", [P, 16, D])
    keysT = din("keysT", [P, 2, P])
    UTh = din("UTh", [P * P, 8 * P])
    Vh = din("Vh", [NEXP, D])
    c_ident = din("c_ident", [P, P])
    c_bands = din("c_bands", [P, 12, P])
    c_trilT = din("c_trilT", [P, P])
    c_iota = din("c_iota", [P, P])
    c_iota16 = din("c_iota16", [P, 8 * 16 * 16])

    out = nc.dram_tensor("out", [n_tok, D], F32, kind="ExternalOutput").ap()
    kind_dbg = "ExternalOutput" if debug else "Internal"
    h_scr = nc.dram_tensor("h_scr", [n_tok, D], F32, kind=kind_dbg).ap()
    jt_scr = nc.dram_tensor("jt_scr", [NT * P, 3 * P], F32, kind=kind_dbg).ap()
    n2t_scr = nc.dram_tensor("n2t_scr", [NG * P, 8 * GT], BF16, kind="Internal").ap()
    NWCOL = INW + 3 * D + 2048
    win_bf = nc.dram_tensor("win_bf", [D, NWCOL], BF16, kind="Internal").ap()
    ut_bf = nc.dram_tensor("ut_bf", [P * P, 8 * P], BF16, kind="Internal").ap()
    v_bf = nc.dram_tensor("v_bf", [NEXP, D], BF16, kind="Internal").ap()

    es_all = ExitStack()
    sch = Sched(nc, es_all)

    def PE(fn, r=(), w=()):
        return sch.op("pe", fn, r, w)

    def ACT(fn, r=(), w=()):
        return sch.op("act", fn, r, w)

    def DVE(fn, r=(), w=()):
        return sch.op("dve", fn, r, w)

    def POOL(fn, r=(), w=()):
        return sch.op("pool", fn, r, w)

    def DMA(fn, r=(), w=(), q="sp"):
        return sch.op(q, fn, r, w, dma=True)

    out_dmas = []

    with es_all:
        es = es_all
        sb = lambda name, shape, dt: es.enter_context(nc.sbuf_tensor(name, list(shape), dt))
        banks = [es.enter_context(nc.psum_tensor("bank%d" % i, [P, 512], F32)) for i in range(8)]
        bank_rr = [0]

        def next_bank(pool=range(8)):
            pool = list(pool)
            b = pool[bank_rr[0] % len(pool)]
            bank_rr[0] += 1
            return b

        ident = sb("ident", [P, P], BF16)
        vec_t = sb("vec_t", [P, 6, 8], F32)
        fgB = sb("fgB", [P, D], F32)
        epsT = sb("epsT", [P, 2], F32)
        POOL(lambda e: e.memset(epsT[:, 0:1], RMS_EPS), w=["epsT"])
        POOL(lambda e: e.memset(epsT[:, 1:2], LN_EPS), w=["epsT"])
        DMA(lambda e: e.dma_start(out=ident[:], in_=c_ident[:, :]), w=["ident"], q="pool")
        ident_f = sb("ident_f", [P, P], F32)
        DMA(lambda e: e.dma_start(out=ident_f[:], in_=c_ident[:, :]), w=["ident_f"])
        DMA(lambda e: e.dma_start(out=vec_t[:], in_=vecs[:, :, :]), w=["vec"])
        DMA(lambda e: e.dma_start(out=fgB[:], in_=fg.partition_broadcast(P)), w=["fgB"])
        G1, G2, GM, PSC, LNG, LNB = range(6)

        for c in range(8):
            DMA(lambda e, c=c: e.dma_start(out=win_bf[c * P:(c + 1) * P, 0:INW], in_=w_in[c * P:(c + 1) * P, :]),
                w=["win_bf"], q="pool")
        for wi, wsrc in enumerate((swo, xwo, wout)):
            for c in range(0, 8, 2):
                DMA(lambda e, c=c, wi=wi, wsrc=wsrc: e.dma_start(
                    out=win_bf[c * P:(c + 2) * P, INW + wi * D:INW + (wi + 1) * D], in_=wsrc[c * P:(c + 2) * P, :]),
                    w=["win_bf"], q="pool")

        def cast_experts(b_lo=0, b_hi=P, dep=()):
            for b in range(b_lo, b_hi):
                DMA(lambda e, b=b: e.dma_start(out=ut_bf[b * P:(b + 1) * P, :], in_=UTh[b * P:(b + 1) * P, :]),
                    r=list(dep), w=["ut_bf"], q="pool")
                DMA(lambda e, b=b: e.dma_start(out=v_bf[b * P:(b + 1) * P, :], in_=Vh[b * P:(b + 1) * P, :]),
                    r=list(dep), w=["v_bf"], q="pool")

        def rms_tile(src, srckey, xn, xnkey, ms, mskey):
            ACT(lambda e: e.activation(out=xn[:], in_=src[:], func=AF.Square, scale=1.0 / 32.0,
                                       accum_out=ms[:, 0:1]),
                r=[srckey], w=[xnkey, mskey])
            ACT(lambda e: e.activation(out=ms[:, 1:2], in_=ms[:, 0:1], func=AF.Sqrt, bias=epsT[:, 0:1], scale=1.0),
                r=[mskey, "epsT"], w=[mskey + "s"])
            DVE(lambda e: e.reciprocal(out=ms[:, 2:3], in_=ms[:, 1:2]), r=[mskey + "s"], w=[mskey + "r"])
            ACT(lambda e: e.activation(out=xn[:], in_=src[:], func=AF.Copy, scale=ms[:, 2:3]),
                r=[srckey, mskey + "r"], w=[xnkey])

        def transpose_tile(xn, xnkey, bA, bB, j, ncol):
            for c in range(8):
                bk = bA if c < 4 else bB
                psb = banks[bk][:].bitcast(BF16)
                o = (c % 4) * ncol + j * P
                PE(lambda e, c=c, psb=psb, o=o: e.transpose(out=psb[:, o:o + P], in_=xn[:, c * P:(c + 1) * P],
                                                           identity=ident[:]),
                   r=[xnkey, "ident"], w=[("ps", bk)])

        def evac_T(dst, dstkey, bA, bB, gidx, ncol):
            for k, bk in enumerate((bA, bB)):
                psb = banks[bk][:].bitcast(BF16)
                DVE(lambda e, k=k, psb=psb: e.tensor_tensor(
                    out=dst[:, 4 * k:4 * k + 4, :],
                    in0=psb[:, 0:4 * ncol].rearrange("p (c t) -> p c t", c=4),
                    in1=vec_t[:, gidx, 4 * k:4 * k + 4].unsqueeze(2).to_broadcast([P, 4, ncol]),
                    op=ALU.mult),
                    r=[("ps", bk), "vec"], w=[dstkey])

        if "A" in phases:
            esA = ExitStack()
            with esA:
                sbA = lambda name, shape, dt: esA.enter_context(nc.sbuf_tensor(name, list(shape), dt))
                bands = sbA("bands", [P, 12, P], BF16)
                WmT = sbA("WmT", [P, 8, P], BF16)
                Cm = sbA("Cm", [P, 8, P], F32)
                pw_t = sbA("pw_t", [P, 8, 256], BF16)
                kT = sbA("kT", [P, 8, NMEM], BF16)
                vmem = sbA("vmem", [P, 2, D], BF16)
                DMA(lambda e: e.dma_start(out=bands[:], in_=c_bands[:, :, :]), w=["bands"], q="pool")
                DMA(lambda e: e.dma_start(out=pw_t[:], in_=pool_w.rearrange("g (k p) d -> p (g k) d", p=P)), w=["pw"], q="pool")

                esS = ExitStack()
                with esS:
                    sbS = lambda name, shape, dt: esS.enter_context(nc.sbuf_tensor(name, list(shape), dt))
                    wsT_f = sbS("wsT_f", [P, 8, P], F32)
                    tril_f = sbS("tril_f", [P, P], F32)
                    BSb = sbS("BSb", [P, 8, P], F32)
                    ones_bf = sbS("ones_bf", [P, P], BF16)
                    wkv_t = sbS("wkv_t", [P, 8, 2 * D], BF16)
                    memf = [sbS("memf%d" % i, [P, D], F32) for i in range(2)]
                    memn = [sbS("memn%d" % i, [P, D], BF16) for i in range(2)]
                    mms = [sbS("mms%d" % i, [P, 4], F32) for i in range(2)]
                    memnT = sbS("memnT", [P, 8, NMEM], BF16)

                    DMA(lambda e: e.dma_start(out=wsT_f[:], in_=wsT[:, :, :]), w=["wsT_f"])
                    DMA(lambda e: e.dma_start(out=tril_f[:], in_=c_trilT[:, :]), w=["tril_f"])
                    DMA(lambda e: e.dma_start(out=BSb[:].rearrange("p h t -> p (h t)"), in_=bs.partition_broadcast(P)), w=["BSb"])
                    DMA(lambda e: e.dma_start(out=wkv_t[:], in_=wkv.rearrange("(c p) n -> p c n", p=P)), w=["wkv"], q="pool")
                    POOL(lambda e: e.memset(ones_bf[:], 1.0), w=["ones"])
                    DVE(lambda e: e.tensor_tensor(out=WmT[:], in0=wsT_f[:],
                                                  in1=tril_f[:].unsqueeze(1).to_broadcast([P, 8, P]), op=ALU.mult),
                        r=["wsT_f", "tril_f"], w=["WmT"])
                    for half in range(2):
                        bk = next_bank()
                        for hh in range(4):
                            h = half * 4 + hh
                            PE(lambda e, h=h, hh=hh, bk=bk: e.matmul(out=banks[bk][:, hh * P:(hh + 1) * P], lhsT=ones_bf[:],
                                                                     rhs=WmT[:, h, :], start=True, stop=True),
                               r=["ones", "WmT"], w=[("ps", bk)])
                        for hh in range(4):
                            h = half * 4 + hh
                            DVE(lambda e, h=h, hh=hh, bk=bk: e.scalar_tensor_tensor(
                                out=Cm[:, h, :], in0=banks[bk][:, hh * P:(hh + 1) * P], scalar=vec_t[:, LNB, h:h + 1],
                                in1=BSb[:, h, :], op0=ALU.mult, op1=ALU.add),
                                r=[("ps", bk), "vec", "BSb"], w=["Cm"])
                    bA, bB = next_bank(), next_bank()
                    for mt in range(2):
                        DMA(lambda e, mt=mt: e.dma_start(out=memf[mt][:], in_=mem[mt * P:(mt + 1) * P, :]), w=[("memf", mt)])
                        rms_tile(memf[mt], ("memf", mt), memn[mt], ("memn", mt), mms[mt], "mms%d" % mt)
                        transpose_tile(memn[mt], ("memn", mt), bA, bB, mt, NMEM)
                    evac_T(memnT, "memnT", bA, bB, GM, NMEM)
                    for hc in range(8):
                        if hc % 2 == 0:
                            bk = next_bank()
                        for c in range(8):
                            PE(lambda e, hc=hc, c=c, bk=bk: e.matmul(
                                out=banks[bk][:, (hc % 2) * NMEM:(hc % 2 + 1) * NMEM],
                                lhsT=wkv_t[:, c, hc * P:(hc + 1) * P], rhs=memnT[:, c, :], start=(c == 0), stop=(c == 7)),
                               r=["wkv", "memnT"], w=[("ps", bk)])
                        if hc % 2 == 1:
                            ACT(lambda e, hc=hc, bk=bk: e.activation(
                                out=kT[:, hc - 1:hc + 1, :], in_=banks[bk][:].rearrange("p (a m) -> p a m", a=2), func=AF.Copy),
                                r=[("ps", bk)], w=["kT"])
                    for mt in range(2):
                        for n in range(2):
                            bk = next_bank()
                            for c in range(8):
                                PE(lambda e, mt=mt, n=n, c=c, bk=bk: e.matmul(
                                    out=banks[bk][:], lhsT=memnT[:, c, mt * P:(mt + 1) * P],
                                    rhs=wkv_t[:, c, D + n * 512:D + (n + 1) * 512], start=(c == 0), stop=(c == 7)),
                                   r=["wkv", "memnT"], w=[("ps", bk)])
                            ACT(lambda e, mt=mt, n=n, bk=bk: e.activation(out=vmem[:, mt, n * 512:(n + 1) * 512],
                                                                         in_=banks[bk][:], func=AF.Copy),
                                r=[("ps", bk)], w=["vmem"])
                    wqT_t = sbS("wqT_t", [P, 16, D], BF16)
                    keysT_t = sbS("keysT_t", [P, 2, P], BF16)
                    Ws_t = sbS("Ws_t", [P, 8, 2048], BF16)
                    DMA(lambda e: e.dma_start(out=wqT_t[:], in_=wqT[:, :, :]), w=["wqT"], q="pool")
                    DMA(lambda e: e.dma_start(out=keysT_t[:], in_=keysT[:, :, :]), w=["keysT"], q="pool")
                    for dc in range(8):
                        for ib in range(4):
                            bk = next_bank()
                            for ii in range(4):
                                i = ib * 4 + ii
                                PE(lambda e, dc=dc, i=i, ii=ii, bk=bk: e.matmul(
                                    out=banks[bk][:, ii * P:(ii + 1) * P], lhsT=wqT_t[:, i, dc * P:(dc + 1) * P],
                                    rhs=keysT_t[:, i % 2, :], start=True, stop=True),
                                   r=["wqT", "keysT"], w=[("ps", bk)])
                            ACT(lambda e, dc=dc, ib=ib, bk=bk: e.activation(out=Ws_t[:, dc, ib * 512:(ib + 1) * 512],
                                                                          in_=banks[bk][:], func=AF.Copy),
                                r=[("ps", bk)], w=["Ws_t"])
                    DMA(lambda e: e.dma_start(out=win_bf[:, INW + 3 * D:NWCOL].rearrange("(c p) n -> p c n", p=P), in_=Ws_t[:]),
                        r=["Ws_t"], w=["win_bf"])
                sch.barrier()

                xbuf = [sbA("xbuf%d" % i, [P, D], F32) for i in range(2)]
                xn = [sbA("xn%d" % i, [P, D], BF16) for i in range(2)]
                ms = [sbA("ms%d" % i, [P, 4], F32) for i in range(2)]
                nT = sbA("nT", [P, 8, GT], BF16)
                NWB = 3
                wb = [sbA("wb%d" % i, [P, 8, 512], BF16) for i in range(NWB)]
                Pr = [sbA("Pr%d" % i, [P, D], BF16) for i in range(4)]
                gv = [sbA("gv%d" % i, [P, D], F32) for i in range(2)]
                lst = [sbA("lst%d" % i, [P, 8], F32) for i in range(2)]
                zt = [sbA("zt%d" % i, [P, D], BF16) for i in range(2)]
                uT = sbA("uT", [P, 8, GT], BF16)
                qT = sbA("qT", [P, 8, GT], BF16)
                gT = sbA("gT", [P, 24, GT], BF16)
                diffT = sbA("diffT", [P, 8, GT], BF16)
                gatedT = sbA("gatedT", [P, 8, GT], BF16)
                tmpS = sbA("tmpS", [P, 4, P], F32)
                smx = [sbA("smx%d" % i, [P, 16], F32) for i in range(2)]
                ex = [sbA("ex%d" % i, [P, 4, NMEM], BF16) for i in range(2)]
                probsT = sbA("probsT", [P, 8, GT], BF16)
                oT = sbA("oT", [P, 8, GT], BF16)
                M32 = sbA("M32", [P, 8, GT], F32)
                tmpM = sbA("tmpM", [P, 2, GT], F32)
                Mb = sbA("Mb", [P, 8, GT], BF16)
                hbuf = [sbA("hbuf%d" % i, [P, D], F32) for i in range(2)]
                pace = sbA("pace", [P, 2], F32)
                iota16 = sbA("iota16", [P, 16], F32)
                DMA(lambda e: e.dma_start(out=iota16[:], in_=c_iota[:, 0:16]), w=["iota16"])
                n2T = sbA("n2T", [P, 8, GT], BF16)
                Sb = [sbA("Sb%d" % i, [P, 2048], F32) for i in range(2)]
                S2 = [sbA("S2_%d" % i, [P, 2048], F32) for i in range(2)]
                Vt = [sbA("Vt%d" % i, [P, 16, 16], F32) for i in range(2)]
                It = [sbA("It%d" % i, [P, 16, 16], U32) for i in range(2)]
                Itf = [sbA("Itf%d" % i, [P, 16, 16], F32) for i in range(2)]
                Bt = [sbA("Bt%d" % i, [P, 8, 16], F32) for i in range(2)]
                CI = [sbA("CI%d" % i, [P, 8, 16], U32) for i in range(2)]
                CIf = [sbA("CIf%d" % i, [P, 3, 128], F32) for i in range(2)]
                CIu = [sbA("CIu%d" % i, [P, 2, 128], U32) for i in range(2)]
                exg = [sbA("exg%d" % i, [P, 128], F32) for i in range(2)]
                zs = [sbA("zs%d" % i, [P, 16], F32) for i in range(2)]
                JB = [sbA("JB%d" % i, [P, 3, 128], F32) for i in range(2)]
                JT = [sbA("JT%d" % i, [P, 3, 128], F32) for i in range(2)]
                n2t_v = n2t_scr.rearrange("(g p) f -> g p f", p=P)
                NWB_G = 24

                win_v = win_bf.rearrange("(c p) n -> p c n", p=P)
                wb_ctr = [0]

                def load_wb(gb):
                    while wb_ctr[0] <= min(gb + NWB - 1, NG * NWB_G - 1):
                        idx = wb_ctr[0]
                        wb_ctr[0] += 1
                        DMA(lambda e, n=idx % NWB_G, slot=idx % NWB: e.dma_start(out=wb[slot][:], in_=win_v[:, :, n * 512:(n + 1) * 512]),
                            r=["win_bf"], w=[("wb", idx % NWB)])
                    return gb % NWB

                bgen = [None]

                def tick(k):
                    if bgen[0] is None:
                        return
                    n = 0
                    while k is None or n < k:
                        try:
                            next(bgen[0])
                        except StopIteration:
                            bgen[0] = None
                            return
                        n += 1

                def routing_gen(t0):
                    for j in range(NJ):
                        T = t0 + j
                        SbK = [("Sb", j, n) for n in range(4)]
                        for i0 in range(0, 16, 4):
                            for i in range(i0, i0 + 4):
                                DVE(lambda e, i=i, j=j: e.max(out=Vt[j][:, i, 0:8], in_=Sb[j][:, i * P:(i + 1) * P]),
                                    r=[("Sb", j, i // 4)], w=[("Vt", j, i, 0)])
                            yield
                        for i0 in range(0, 16, 4):
                            for i in range(i0, i0 + 4):
                                DVE(lambda e, i=i, j=j: e.max_index(out=It[j][:, i, 0:8], in_max=Vt[j][:, i, 0:8],
                                                                    in_values=Sb[j][:, i * P:(i + 1) * P]),
                                    r=[("Sb", j, i // 4), ("Vt", j, i, 0)], w=[("It", j, i, 0)])
                            yield
                        for i0 in range(0, 16, 4):
                            for i in range(i0, i0 + 4):
                                DVE(lambda e, i=i, j=j: e.match_replace(out=S2[j][:, i * P:(i + 1) * P], in_to_replace=Vt[j][:, i, 0:8],
                                                                        in_values=Sb[j][:, i * P:(i + 1) * P], imm_value=NEG),
                                    r=[("Sb", j, i // 4), ("Vt", j, i, 0)], w=[("S2", j, i)])
                            yield
                        for i0 in range(0, 16, 4):
                            for i in range(i0, i0 + 4):
                                DVE(lambda e, i=i, j=j: e.max(out=Vt[j][:, i, 8:16], in_=S2[j][:, i * P:(i + 1) * P]),
                                    r=[("S2", j, i)], w=[("Vt", j, i, 1)])
                            yield
                        for i0 in range(0, 16, 4):
                            for i in range(i0, i0 + 4):
                                DVE(lambda e, i=i, j=j: e.max_index(out=It[j][:, i, 8:16], in_max=Vt[j][:, i, 8:16],
                                                                    in_values=S2[j][:, i * P:(i + 1) * P]),
                                    r=[("S2", j, i), ("Vt", j, i, 1)], w=[("It", j, i, 1)])
                            yield
                        allV = [("Vt", j, i, k) for i in range(16) for k in range(2)]
                        allI = [("It", j, i, k) for i in range(16) for k in range(2)]
                        DVE(lambda e, j=j: e.tensor_copy(out=Itf[j][:], in_=It[j][:]), r=allI, w=[("Itf", j)])
                        cand = Sb[j][:].rearrange("p (h c) -> p h c", h=8)
                        cand2 = S2[j][:].rearrange("p (h c) -> p h c", h=8)
                        Vv = Vt[j][:].rearrange("p (h f) k -> p h f k", f=2)
                        DVE(lambda e, cand=cand, Vv=Vv: e.tensor_tensor(
                            out=cand.rearrange("p h (a b) -> p h a b", a=16),
                            in0=Vv[:, :, 0, :].unsqueeze(3).to_broadcast([P, 8, 16, 16]),
                            in1=Vv[:, :, 1, :].unsqueeze(2).to_broadcast([P, 8, 16, 16]), op=ALU.add),
                            r=allV + SbK, w=SbK)
                        yield
                        for h0 in range(0, 8, 4):
                            for h in range(h0, h0 + 4):
                                DVE(lambda e, h=h, j=j, cand=cand: e.max(out=Bt[j][:, h, 0:8], in_=cand[:, h, :]),
                                    r=SbK, w=[("Bt", j, h, 0)])
                            yield
                        for h0 in range(0, 8, 4):
                            for h in range(h0, h0 + 4):
                                DVE(lambda e, h=h, j=j, cand=cand: e.max_index(out=CI[j][:, h, 0:8], in_max=Bt[j][:, h, 0:8],
                                                                               in_values=cand[:, h, :]),
                                    r=SbK + [("Bt", j, h, 0)], w=[("CI", j, h, 0)])
                            yield
                        for h0 in range(0, 8, 4):
                            for h in range(h0, h0 + 4):
                                DVE(lambda e, h=h, j=j, cand=cand, cand2=cand2: e.match_replace(
                                    out=cand2[:, h, :], in_to_replace=Bt[j][:, h, 0:8], in_values=cand[:, h, :], imm_value=NEG),
                                    r=SbK + [("Bt", j, h, 0)], w=[("S2", j, 2 * h), ("S2", j, 2 * h + 1)])
                            yield
                        for h0 in range(0, 8, 4):
                            for h in range(h0, h0 + 4):
                                DVE(lambda e, h=h, j=j, cand2=cand2: e.max(out=Bt[j][:, h, 8:16], in_=cand2[:, h, :]),
                                    r=[("S2", j, 2 * h), ("S2", j, 2 * h + 1)], w=[("Bt", j, h, 1)])
                            yield
                        for h0 in range(0, 8, 4):
                            for h in range(h0, h0 + 4):
                                DVE(lambda e, h=h, j=j, cand2=cand2: e.max_index(out=CI[j][:, h, 8:16], in_max=Bt[j][:, h, 8:16],
                                                                                in_values=cand2[:, h, :]),
                                    r=[("S2", j, 2 * h), ("S2", j, 2 * h + 1), ("Bt", j, h, 1)], w=[("CI", j, h, 1)])
                            yield
                        allB = [("Bt", j, h, k) for h in range(8) for k in range(2)]
                        allC = [("CI", j, h, k) for h in range(8) for k in range(2)]
                        POOL(lambda e, j=j: e.tensor_tensor(
                            out=exg[j][:].rearrange("p (h k) -> p h k", h=8), in0=Bt[j][:],
                            in1=Bt[j][:, :, 0:1].to_broadcast([P, 8, 16]), op=ALU.subtract),
                            r=allB, w=[("exg", j)])
                        ACT(lambda e, j=j: e.activation(out=exg[j][:], in_=exg[j][:], func=AF.Exp),
                            r=[("exg", j)], w=[("exg", j)])
                        DVE(lambda e, j=j: e.tensor_reduce(out=zs[j][:, 0:8], in_=exg[j][:].rearrange("p (h k) -> p h k", h=8),
                                                           axis=AX.X, op=ALU.add),
                            r=[("exg", j)], w=[("zs", j)])
                        DVE(lambda e, j=j: e.reciprocal(out=zs[j][:, 8:16], in_=zs[j][:, 0:8]), r=[("zs", j)], w=[("zr", j)])
                        POOL(lambda e, j=j: e.tensor_tensor(
                            out=JB[j][:, 2, :].rearrange("p (h k) -> p h k", h=8),
                            in0=exg[j][:].rearrange("p (h k) -> p h k", h=8),
                            in1=zs[j][:, 8:16].unsqueeze(2).to_broadcast([P, 8, 16]), op=ALU.mult),
                            r=[("exg", j), ("zr", j)], w=[("JB", j, 2)])
                        yield
                        DVE(lambda e, j=j: e.tensor_single_scalar(out=CIu[j][:, 0, :], in_=CI[j][:].rearrange("p h k -> p (h k)"),
                                                                  scalar=15, op=ALU.bitwise_and),
                            r=allC, w=[("CIu", j, 0)])
                        DVE(lambda e, j=j: e.tensor_single_scalar(out=CIu[j][:, 1, :], in_=CI[j][:].rearrange("p h k -> p (h k)"),
                                                                  scalar=4, op=ALU.logical_shift_right),
                            r=allC, w=[("CIu", j, 1)])
                        DVE(lambda e, j=j: e.tensor_copy(out=CIf[j][:, 1, :], in_=CIu[j][:, 0, :]),
                            r=[("CIu", j, 0)], w=[("CIf", j, 1)])
                        DVE(lambda e, j=j: e.tensor_copy(out=CIf[j][:, 2, :], in_=CIu[j][:, 1, :]),
                            r=[("CIu", j, 1)], w=[("CIf", j, 2)])
                        yield
                        Iv = Itf[j][:].rearrange("p (h f) k -> p h f k", f=2)
                        specs = ((0, 2, 0, Sb[j], SbK), (1, 1, 1, S2[j], [("S2", j, i) for i in range(16)]))
                        for which, row, half, mbuf, mkeys in specs:
                            mk = mbuf[:].rearrange("p (h q a) -> p h q a", h=8, q=16)
                            DVE(lambda e, j=j, row=row, mk=mk: e.tensor_tensor(
                                out=mk, in0=iota16[:].unsqueeze(1).unsqueeze(1).to_broadcast([P, 8, 16, 16]),
                                in1=CIf[j][:, row, :].rearrange("p (h q) -> p h q", h=8).unsqueeze(3).to_broadcast([P, 8, 16, 16]),
                                op=ALU.is_equal),
                                r=[("CIf", j, row), "iota16"] + mkeys, w=mkeys)
                            POOL(lambda e, half=half, mk=mk, Iv=Iv: e.tensor_tensor(
                                out=mk, in0=mk, in1=Iv[:, :, half, :].unsqueeze(2).to_broadcast([P, 8, 16, 16]), op=ALU.mult),
                                r=mkeys + [("Itf", j)], w=mkeys)
                            yield
                        yield
                        for which, row, half, mbuf, mkeys in specs:
                            mk = mbuf[:].rearrange("p (h q a) -> p h q a", h=8, q=16)
                            DVE(lambda e, j=j, which=which, mk=mk: e.tensor_reduce(
                                out=JB[j][:, which, :].rearrange("p (h q) -> p h q", h=8), in_=mk, axis=AX.X, op=ALU.add),
                                r=mkeys, w=[("JB", j, which)])
                        yield
                        bk = next_bank()
                        for w3 in range(3):
                            PE(lambda e, w3=w3, j=j, bk=bk: e.transpose(out=banks[bk][:, w3 * P:(w3 + 1) * P], in_=JB[j][:, w3, :],
                                                                       identity=ident_f[:]),
                               r=[("JB", j, w3), "ident_f"], w=[("ps", bk)])
                        ACT(lambda e, j=j, bk=bk: e.activation(out=JT[j][:].rearrange("p w t -> p (w t)"), in_=banks[bk][:, 0:3 * P],
                                                               func=AF.Copy),
                            r=[("ps", bk)], w=[("JT", j)])
                        DMA(lambda e, T=T, j=j: e.dma_start(out=jt_scr[T * P:(T + 1) * P, :], in_=JT[j][:].rearrange("p w t -> p (w t)")),
                            r=[("JT", j)], w=["jt_scr"])
                        yield

                xslot = [0]
                for g in range(NG):
                    t0 = g * NJ
                    bA, bB = next_bank(), next_bank()
                    for j in range(NJ):
                        T = t0 + j
                        sl = xslot[0] % 2
                        xslot[0] += 1
                        DMA(lambda e, T=T, sl=sl: e.dma_start(out=xbuf[sl][:], in_=x[T * P:(T + 1) * P, :]), w=[("xbuf", sl)])
                        tick(3)
                        rms_tile(xbuf[sl], ("xbuf", sl), xn[sl], ("xn", sl), ms[sl], "ms%d" % sl)
                        transpose_tile(xn[sl], ("xn", sl), bA, bB, j, GT)
                    tick(3)
                    evac_T(nT, "nT", bA, bB, G1, GT)
                    DVE(lambda e: e.tensor_copy(out=pace[:, 0:1], in_=epsT[:, 0:1]), r=["epsT"], w=[("pace", g)])
                    nb_ = P // NG
                    cast_experts(g * nb_, (g + 1) * nb_, dep=[("pace", g)])

                    for n in range(14):
                        slot = load_wb(g * NWB_G + n)
                        tick(4)
                        if n in (0, 1, 4, 5):
                            for j in range(NJ):
                                T = t0 + j
                                bk = next_bank()
                                for c in range(8):
                                    PE(lambda e, c=c, j=j, bk=bk, slot=slot: e.matmul(
                                        out=banks[bk][:], lhsT=nT[:, c, j * P:(j + 1) * P], rhs=wb[slot][:, c, :],
                                        start=(c == 0), stop=(c == 7)),
                                       r=["nT", ("wb", slot)], w=[("ps", bk)])
                                if n < 2:
                                    ACT(lambda e, T=T, n=n, bk=bk: e.activation(
                                        out=Pr[T % 4][:, n * 512:(n + 1) * 512], in_=banks[bk][:], func=AF.Copy),
                                        r=[("ps", bk)], w=[("Pr", T % 4)])
                                else:
                                    nn = n - 4
                                    ACT(lambda e, j=j, nn=nn, bk=bk: e.activation(
                                        out=gv[j][:, nn * 512:(nn + 1) * 512], in_=banks[bk][:], func=AF.Gelu,
                                        accum_out=lst[j][:, nn:nn + 1]),
                                        r=[("ps", bk)], w=[("gv", j), ("lst", j, nn)])
                        else:
                            for kk in range(2):
                                bk = next_bank()
                                for k2 in range(2):
                                    k = kk * 2 + k2
                                    for c in range(8):
                                        PE(lambda e, c=c, k=k, k2=k2, bk=bk, slot=slot: e.matmul(
                                            out=banks[bk][:, k2 * GT:(k2 + 1) * GT], lhsT=wb[slot][:, c, k * P:(k + 1) * P],
                                            rhs=nT[:, c, :], start=(c == 0), stop=(c == 7)),
                                           r=["nT", ("wb", slot)], w=[("ps", bk)])
                                src = banks[bk][:].rearrange("p (a t) -> p a t", a=2)
                                if n in (2, 3):
                                    ch = (n - 2) * 4 + kk * 2
                                    ACT(lambda e, ch=ch, src=src: e.activation(out=uT[:, ch:ch + 2, :], in_=src, func=AF.Gelu),
                                        r=[("ps", bk)], w=["uT"])
                                elif n in (6, 7):
                                    ch = (n - 6) * 4 + kk * 2
                                    ACT(lambda e, ch=ch, src=src: e.activation(out=qT[:, ch:ch + 2, :], in_=src, func=AF.Copy),
                                        r=[("ps", bk)], w=["qT"])
                                else:
                                    ch = (n - 8) * 4 + kk * 2
                                    ACT(lambda e, ch=ch, src=src: e.activation(out=gT[:, ch:ch + 2, :], in_=src, func=AF.Sigmoid),
                                        r=[("ps", bk)], w=["gT"])
                        if n == 5:
                            for j in range(NJ):
                                ACT(lambda e, j=j: e.activation(out=zt[j][:], in_=gv[j][:], func=AF.Square,
                                                                accum_out=lst[j][:, 2:3]),
                                    r=[("gv", j)], w=[("zt", j), ("lst", j, 2)])
                                tick(1)
                                DVE(lambda e, j=j: e.tensor_scalar(out=lst[j][:, 3:4], in0=lst[j][:, 0:1], scalar1=lst[j][:, 1:2],
                                                                   scalar2=1.0 / D, op0=ALU.add, op1=ALU.mult),
                                    r=[("lst", j, 0), ("lst", j, 1)], w=[("lst", j, 3)])
                                DVE(lambda e, j=j: e.tensor_tensor(out=lst[j][:, 4:5], in0=lst[j][:, 3:4], in1=lst[j][:, 3:4],
                                                                   op=ALU.mult),
                                    r=[("lst", j, 3)], w=[("lst", j, 4)])
                                DVE(lambda e, j=j: e.scalar_tensor_tensor(out=lst[j][:, 5:6], in0=lst[j][:, 2:3], scalar=1.0 / D,
                                                                          in1=lst[j][:, 4:5], op0=ALU.mult, op1=ALU.subtract),
                                    r=[("lst", j, 2), ("lst", j, 4)], w=[("lst", j, 5)])
                                ACT(lambda e, j=j: e.activation(out=lst[j][:, 7:8], in_=lst[j][:, 5:6], func=AF.Sqrt,
                                                                bias=epsT[:, 1:2], scale=1.0),
                                    r=[("lst", j, 5), "epsT"], w=[("lst", j, 7)])
                                DVE(lambda e, j=j: e.reciprocal(out=lst[j][:, 6:7], in_=lst[j][:, 7:8]),
                                    r=[("lst", j, 7)], w=[("lst", j, 6)])
                                DVE(lambda e, j=j: e.tensor_scalar(out=zt[j][:], in0=gv[j][:], scalar1=lst[j][:, 3:4],
                                                                   scalar2=lst[j][:, 6:7], op0=ALU.subtract, op1=ALU.mult),
                                    r=[("gv", j), ("lst", j, 3), ("lst", j, 6)], w=[("zt", j)])

                    for j in range(NJ):
                        T = t0 + j
                        for half in range(2):
                            bk = next_bank()
                            for cc in range(4):
                                c = half * 4 + cc
                                gi = c // 2
                                band0 = (8 + gi) if T == 0 else gi
                                PE(lambda e, c=c, cc=cc, T=T, bk=bk, band0=band0: e.matmul(
                                    out=banks[bk][:, cc * P:(cc + 1) * P], lhsT=Pr[T % 4][:, c * P:(c + 1) * P],
                                    rhs=bands[:, band0, :], start=True, stop=(T == 0)),
                                   r=[("Pr", T % 4), "bands"], w=[("ps", bk)])
                                if T > 0:
                                    PE(lambda e, c=c, cc=cc, T=T, bk=bk, gi=gi: e.matmul(
                                        out=banks[bk][:, cc * P:(cc + 1) * P], lhsT=Pr[(T - 1) % 4][:, c * P:(c + 1) * P],
                                        rhs=bands[:, 4 + gi, :], start=False, stop=True),
                                       r=[("Pr", (T - 1) % 4), "bands"], w=[("ps", bk)])
                            ACT(lambda e, half=half, j=j, bk=bk: e.activation(
                                out=diffT[:, half * 4:half * 4 + 4, j * P:(j + 1) * P],
                                in_=banks[bk][:].rearrange("p (c t) -> p c t", c=4), func=AF.Copy),
                                r=[("ps", bk)], w=["diffT"])
                    for dc in range(8):
                        if dc % 2 == 0:
                            bk = next_bank()
                        gi = dc // 2
                        for kc in range(2):
                            PE(lambda e, dc=dc, gi=gi, kc=kc, bk=bk: e.matmul(
                                out=banks[bk][:, (dc % 2) * GT:(dc % 2 + 1) * GT],
                                lhsT=pw_t[:, gi * 2 + kc, (dc % 2) * P:(dc % 2 + 1) * P], rhs=diffT[:, gi * 2 + kc, :],
                                start=(kc == 0), stop=(kc == 1)),
                               r=["pw", "diffT"], w=[("ps", bk)])
                        tick(1)
                        DVE(lambda e, dc=dc, bk=bk: e.scalar_tensor_tensor(
                            out=M32[:, dc, :], in0=banks[bk][:, (dc % 2) * GT:(dc % 2 + 1) * GT],
                            scalar=vec_t[:, PSC, dc:dc + 1], in1=gT[:, dc, :], op0=ALU.mult, op1=ALU.mult),
                            r=[("ps", bk), "vec", "gT"], w=["M32"])

                    for j in range(NJ):
                        for half in range(2):
                            bk = next_bank()
                            for hh in range(4):
                                h = half * 4 + hh
                                PE(lambda e, h=h, hh=hh, j=j, bk=bk: e.matmul(
                                    out=banks[bk][:, hh * P:(hh + 1) * P], lhsT=zt[j][:, h * P:(h + 1) * P],
                                    rhs=WmT[:, h, :], start=True, stop=True),
                                   r=[("zt", j), "WmT"], w=[("ps", bk)])
                            h0 = half * 4
                            tick(1)
                            DVE(lambda e, h0=h0, bk=bk: e.tensor_tensor(
                                out=tmpS[:], in0=banks[bk][:].rearrange("p (h t) -> p h t", h=4),
                                in1=vec_t[:, LNG, h0:h0 + 4].unsqueeze(2).to_broadcast([P, 4, P]), op=ALU.mult),
                                r=[("ps", bk), "vec"], w=["tmpS"])
                            DVE(lambda e, h0=h0: e.tensor_tensor(out=tmpS[:], in0=tmpS[:], in1=Cm[:, h0:h0 + 4, :], op=ALU.add),
                                r=["tmpS", "Cm"], w=["tmpS"])
                            DVE(lambda e, h0=h0, j=j: e.tensor_tensor(
                                out=gatedT[:, h0:h0 + 4, j * P:(j + 1) * P], in0=tmpS[:],
                                in1=uT[:, h0:h0 + 4, j * P:(j + 1) * P], op=ALU.mult),
                                r=["tmpS", "uT"], w=["gatedT"])
                    for dcp in range(4):
                        if dcp % 2 == 0:
                            slot = load_wb(g * NWB_G + 14 + dcp // 2)
                        tick(2)
                        bk = next_bank()
                        for d2 in range(2):
                            dc = dcp * 2 + d2
                            for hc in range(8):
                                PE(lambda e, dc=dc, d2=d2, hc=hc, bk=bk, slot=slot: e.matmul(
                                    out=banks[bk][:, d2 * GT:(d2 + 1) * GT], lhsT=wb[slot][:, hc, (dc % 4) * P:(dc % 4 + 1) * P],
                                    rhs=gatedT[:, hc, :], start=(hc == 0), stop=(hc == 7)),
                                   r=[("wb", slot), "gatedT"], w=[("ps", bk)])
                        dc0 = dcp * 2
                        tick(1)
                        DVE(lambda e, dc0=dc0, bk=bk: e.tensor_tensor(
                            out=tmpM[:], in0=banks[bk][:].rearrange("p (a t) -> p a t", a=2),
                            in1=gT[:, 8 + dc0:8 + dc0 + 2, :], op=ALU.mult),
                            r=[("ps", bk), "gT"], w=["tmpM"])
                        DVE(lambda e, dc0=dc0: e.tensor_tensor(out=M32[:, dc0:dc0 + 2, :], in0=M32[:, dc0:dc0 + 2, :],
                                                               in1=tmpM[:], op=ALU.add),
                            r=["tmpM", "M32"], w=["M32"])

                    for j in range(NJ):
                        bks = [next_bank(), next_bank()]
                        for a in range(4):
                            bk = bks[a // 2]
                            for kc in range(2):
                                PE(lambda e, a=a, kc=kc, j=j, bk=bk: e.matmul(
                                    out=banks[bk][:, (a % 2) * NMEM:(a % 2 + 1) * NMEM],
                                    lhsT=qT[:, 2 * a + kc, j * P:(j + 1) * P], rhs=kT[:, 2 * a + kc, :],
                                    start=(kc == 0), stop=(kc == 1)),
                                   r=["qT", "kT"], w=[("ps", bk)])
                        for hb_ in range(2):
                            bk = bks[hb_]
                            tick(1)
                            DVE(lambda e, j=j, hb_=hb_, bk=bk: e.tensor_reduce(
                                out=smx[j][:, hb_ * 2:hb_ * 2 + 2], in_=banks[bk][:].rearrange("p (a m) -> p a m", a=2),
                                axis=AX.X, op=ALU.max),
                                r=[("ps", bk)], w=[("smx", j, hb_)])
                            DVE(lambda e, j=j, hb_=hb_: e.tensor_scalar(
                                out=smx[j][:, 4 + hb_ * 2:4 + hb_ * 2 + 2], in0=smx[j][:, hb_ * 2:hb_ * 2 + 2],
                                scalar1=-1.0 / 16.0, scalar2=None, op0=ALU.mult),
                                r=[("smx", j, hb_)], w=[("smxn", j, hb_)])
                        for a in range(4):
                            bk = bks[a // 2]
                            ACT(lambda e, a=a, j=j, bk=bk: e.activation(
                                out=ex[j][:, a, :], in_=banks[bk][:, (a % 2) * NMEM:(a % 2 + 1) * NMEM], func=AF.Exp,
                                bias=smx[j][:, 4 + a:5 + a], scale=1.0 / 16.0, accum_out=smx[j][:, 8 + a:9 + a]),
                                r=[("ps", bk), ("smxn", j, a // 2)], w=[("ex", j), ("sse", j, a)])
                        tick(1)
                        DVE(lambda e, j=j: e.reciprocal(out=smx[j][:, 12:16], in_=smx[j][:, 8:12]),
                            r=[("sse", j, a) for a in range(4)], w=[("srs", j)])
                        DVE(lambda e, j=j: e.tensor_tensor(
                            out=ex[j][:], in0=ex[j][:], in1=smx[j][:, 12:16].unsqueeze(2).to_broadcast([P, 4, NMEM]),
                            op=ALU.mult),
                            r=[("ex", j), ("srs", j)], w=[("ex", j)])
                        bk = next_bank()
                        psb = banks[bk][:].bitcast(BF16)
                        for a in range(4):
                            for mc in range(2):
                                o = (a * 2 + mc) * P
                                PE(lambda e, a=a, mc=mc, o=o, j=j, psb=psb: e.transpose(
                                    out=psb[:, o:o + P], in_=ex[j][:, a, mc * P:(mc + 1) * P], identity=ident[:]),
                                   r=[("ex", j), "ident"], w=[("ps", bk)])
                        ACT(lambda e, j=j, psb=psb: e.activation(
                            out=probsT[:, :, j * P:(j + 1) * P], in_=psb[:].rearrange("p (c t) -> p c t", c=8), func=AF.Copy),
                            r=[("ps", bk)], w=["probsT"])
                    for hcp in range(4):
                        bk = next_bank()
                        for h2 in range(2):
                            hc = hcp * 2 + h2
                            a = hc // 2
                            for mc in range(2):
                                PE(lambda e, hc=hc, h2=h2, a=a, mc=mc, bk=bk: e.matmul(
                                    out=banks[bk][:, h2 * GT:(h2 + 1) * GT], lhsT=vmem[:, mc, hc * P:(hc + 1) * P],
                                    rhs=probsT[:, a * 2 + mc, :], start=(mc == 0), stop=(mc == 1)),
                                   r=["vmem", "probsT"], w=[("ps", bk)])
                        ACT(lambda e, hcp=hcp, bk=bk: e.activation(
                            out=oT[:, hcp * 2:hcp * 2 + 2, :], in_=banks[bk][:].rearrange("p (a t) -> p a t", a=2), func=AF.Copy),
                            r=[("ps", bk)], w=["oT"])
                    for dcp in range(4):
                        if dcp % 2 == 0:
                            slot = load_wb(g * NWB_G + 16 + dcp // 2)
                        tick(2)
                        bk = next_bank()
                        for d2 in range(2):
                            dc = dcp * 2 + d2
                            for hc in range(8):
                                PE(lambda e, dc=dc, d2=d2, hc=hc, bk=bk, slot=slot: e.matmul(
                                    out=banks[bk][:, d2 * GT:(d2 + 1) * GT], lhsT=wb[slot][:, hc, (dc % 4) * P:(dc % 4 + 1) * P],
                                    rhs=oT[:, hc, :], start=(hc == 0), stop=(hc == 7)),
                                   r=[("wb", slot), "oT"], w=[("ps", bk)])
                        dc0 = dcp * 2
                        tick(1)
                        DVE(lambda e, dc0=dc0, bk=bk: e.tensor_tensor(
                            out=tmpM[:], in0=banks[bk][:].rearrange("p (a t) -> p a t", a=2),
                            in1=gT[:, 16 + dc0:16 + dc0 + 2, :], op=ALU.mult),
                            r=[("ps", bk), "gT"], w=["tmpM"])
                        DVE(lambda e, dc0=dc0: e.tensor_tensor(out=Mb[:, dc0:dc0 + 2, :], in0=M32[:, dc0:dc0 + 2, :],
                                                               in1=tmpM[:], op=ALU.add),
                            r=["tmpM", "M32"], w=["Mb"])

                    xsl = []
                    for j in range(NJ):
                        T = t0 + j
                        sl = xslot[0] % 2
                        xslot[0] += 1
                        xsl.append(sl)
                        DMA(lambda e, T=T, sl=sl: e.dma_start(out=xbuf[sl][:], in_=x[T * P:(T + 1) * P, :]), w=[("xbuf", sl)])
                    for n in range(2):
                        slot = load_wb(g * NWB_G + 18 + n)
                        tick(2)
                        for j in range(NJ):
                            sl = xsl[j]
                            bk = next_bank()
                            for c in range(8):
                                PE(lambda e, c=c, j=j, bk=bk, slot=slot: e.matmul(
                                    out=banks[bk][:], lhsT=Mb[:, c, j * P:(j + 1) * P], rhs=wb[slot][:, c, :],
                                    start=(c == 0), stop=(c == 7)),
                                   r=["Mb", ("wb", slot)], w=[("ps", bk)])
                            tick(1)
                            DVE(lambda e, j=j, n=n, bk=bk, sl=sl: e.tensor_tensor(
                                out=hbuf[j][:, n * 512:(n + 1) * 512], in0=banks[bk][:], in1=xbuf[sl][:, n * 512:(n + 1) * 512],
                                op=ALU.add),
                                r=[("ps", bk), ("xbuf", sl)], w=[("hbuf", j)])
                    for j in range(NJ):
                        T = t0 + j
                        DMA(lambda e, T=T, j=j: e.dma_start(out=h_scr[T * P:(T + 1) * P, :], in_=hbuf[j][:]),
                            r=[("hbuf", j)], w=["h_scr"])
                    tick(None)
                    bA, bB = next_bank(), next_bank()
                    for j in range(NJ):
                        rms_tile(hbuf[j], ("hbuf", j), xn[j], ("xn", j), ms[j], "ms%d" % j)
                        transpose_tile(xn[j], ("xn", j), bA, bB, j, GT)
                    evac_T(n2T, "n2T", bA, bB, G2, GT)
                    DMA(lambda e, g=g: e.dma_start(out=n2t_v[g], in_=n2T[:].rearrange("p c t -> p (c t)")),
                        r=["n2T"], w=["n2t_scr"])
                    for n in range(4):
                        slot = load_wb(g * NWB_G + 20 + n)
                        for j in range(NJ):
                            bk = next_bank()
                            for c in range(8):
                                PE(lambda e, c=c, j=j, bk=bk, slot=slot: e.matmul(
                                    out=banks[bk][:], lhsT=n2T[:, c, j * P:(j + 1) * P], rhs=wb[slot][:, c, :],
                                    start=(c == 0), stop=(c == 7)),
                                   r=["n2T", ("wb", slot)], w=[("ps", bk)])
                            ACT(lambda e, j=j, n=n, bk=bk: e.activation(out=Sb[j][:, n * 512:(n + 1) * 512], in_=banks[bk][:],
                                                                        func=AF.Copy),
                                r=[("ps", bk)], w=[("Sb", j, n)])
                    bgen[0] = routing_gen(t0)
                tick(None)
            sch.barrier()
        else:
            cast_experts()

        if "B2" in phases:
            esC = ExitStack()
            with esC:
                sbC = lambda name, shape, dt: esC.enter_context(nc.sbuf_tensor(name, list(shape), dt))
                iotaE = sbC("iotaE", [P, P], BF16)
                DMA(lambda e: e.dma_start(out=iotaE[:], in_=c_iota[:, :]), w=["iotaE"], q="pool")
                hbC = sbC("hbC", [P, D], F32)
                n2TC = [sbC("n2TC%d" % i, [P, 8, GT], BF16) for i in range(2)]
                JTC = [sbC("JTC%d" % i, [P, 3, P], F32) for i in range(4)]
                TP = 8
                NOH = 2
                OH1s = [sbC("OH1s%d" % i, [P, TP, P], BF16) for i in range(NOH)]
                OH2s = [sbC("OH2s%d" % i, [P, TP, P], BF16) for i in range(NOH)]
                Gf = [sbC("Gf%d" % i, [P, GT, P], BF16) for i in range(2)]
                NUB, NVB = 3, 4
                ub = [sbC("ub%d" % i, [P, 2, 8, P], BF16) for i in range(NUB)]
                vb = [sbC("vb%d" % i, [P, 2, D], BF16) for i in range(NVB)]
                NHA = 3
                Hg = [sbC("Hg%d" % i, [P, 2, GT], BF16) for i in range(NHA)]
                At = [sbC("At%d" % i, [P, 2, GT], BF16) for i in range(NHA)]
                ms3 = sbC("ms3", [P, 4], F32)
                ob = sbC("ob", [P, D], F32)

                n2t_v = n2t_scr.rearrange("(g p) f -> g p f", p=P)
                ut_v = ut_bf.rearrange("(b p) (c e) -> p b c e", p=P, c=8)
                v_v = v_bf.rearrange("(b e) d -> e b d", e=P)
                YB = [[0, 1], [2, 3]]
                HB = [4, 5]
                MB = [6, 7]
                NP2 = P // 2
                uq = [0]
                vq = [0]
                ohc = [0]

                def load_group_inputs(gg):
                    par = gg % 2
                    DMA(lambda e, gg=gg, par=par: e.dma_start(out=n2TC[par][:].rearrange("p c t -> p (c t)"), in_=n2t_v[gg]),
                        r=["n2t_scr"], w=[("n2TC", par)])
                    for j in range(NJ):
                        T = gg * NJ + j
                        DMA(lambda e, T=T, j=j, par=par: e.dma_start(out=JTC[par * 2 + j][:].rearrange("p w t -> p (w t)"),
                                                                     in_=jt_scr[T * P:(T + 1) * P, :]),
                            r=["jt_scr"], w=[("JTC", par * 2 + j)])

                def prefetch_u(hi):
                    while uq[0] <= min(hi, NG * NP2 - 1):
                        idx = uq[0]
                        uq[0] += 1
                        b0 = (idx % NP2) * 2
                        DMA(lambda e, b0=b0, us=idx % NUB: e.dma_start(out=ub[us][:], in_=ut_v[:, b0:b0 + 2, :, :]),
                            r=["ut_bf"], w=[("ub", idx % NUB)])

                def prefetch_v(hi):
                    while vq[0] <= min(hi, NG * NP2 - 1):
                        idx = vq[0]
                        vq[0] += 1
                        b0 = (idx % NP2) * 2
                        DMA(lambda e, b0=b0, vs=idx % NVB: e.dma_start(out=vb[vs][:], in_=v_v[:, b0:b0 + 2, :]),
                            r=["v_bf"], w=[("vb", idx % NVB)])

                def build_steps(gg):
                    par = gg % 2
                    Gd = Gf[par]
                    deferred = [None]
                    for j in range(NJ):
                        jt = JTC[par * 2 + j]
                        jk = ("JTC", par * 2 + j)
                        for pc in range(P // TP):
                            sl = ohc[0] % NOH
                            ohc[0] += 1
                            tq = pc * TP
                            iob = iotaE[:].unsqueeze(1).to_broadcast([P, TP, P])
                            DVE(lambda e, sl=sl, tq=tq, iob=iob, jt=jt: e.tensor_tensor(
                                out=OH1s[sl][:], in0=iob, in1=jt[:, 0, tq:tq + TP].unsqueeze(2).to_broadcast([P, TP, P]),
                                op=ALU.is_equal),
                                r=["iotaE", jk], w=[("OH1", sl)])
                            DVE(lambda e, sl=sl, tq=tq, iob=iob, jt=jt: e.tensor_tensor(
                                out=OH2s[sl][:], in0=iob, in1=jt[:, 1, tq:tq + TP].unsqueeze(2).to_broadcast([P, TP, P]),
                                op=ALU.is_equal),
                                r=["iotaE", jk], w=[("OH2", sl)])
                            POOL(lambda e, sl=sl, tq=tq, jt=jt: e.tensor_tensor(
                                out=OH2s[sl][:], in0=OH2s[sl][:], in1=jt[:, 2, tq:tq + TP].unsqueeze(2).to_broadcast([P, TP, P]),
                                op=ALU.mult),
                                r=[("OH2", sl), jk], w=[("OH2", sl)])
                            def pe_part(sl=sl, j=j, tq=tq, Gd=Gd, par=par):
                                for q4 in range(TP // 4):
                                    bk = next_bank(MB)
                                    for tt in range(4):
                                        tl = q4 * 4 + tt
                                        PE(lambda e, sl=sl, tl=tl, tt=tt, bk=bk: e.matmul(
                                            out=banks[bk][:, tt * P:(tt + 1) * P], lhsT=OH2s[sl][:, tl, :], rhs=OH1s[sl][:, tl, :],
                                            start=True, stop=True),
                                           r=[("OH1", sl), ("OH2", sl)], w=[("ps", bk)])
                                    tok = j * P + tq + q4 * 4
                                    ACT(lambda e, tok=tok, bk=bk, Gd=Gd: e.activation(
                                        out=Gd[:, tok:tok + 4, :], in_=banks[bk][:].rearrange("p (t e) -> p t e", t=4), func=AF.Copy),
                                        r=[("ps", bk)], w=[("Gf", par)])
                            if deferred[0] is not None:
                                deferred[0]()
                            deferred[0] = pe_part
                            yield
                    if deferred[0] is not None:
                        deferred[0]()
                        deferred[0] = None
                load_group_inputs(0)
                prefetch_u(1)
                prefetch_v(1)
                for _ in build_steps(0):
                    pass
                LAG = 2
                for g in range(NG):
                    t0 = g * NJ
                    par = g % 2
                    bg = None
                    if g + 1 < NG:
                        load_group_inputs(g + 1)
                        bg = build_steps(g + 1)
                    nsteps = NJ * (P // TP)
                    done_steps = 0
                    pend = []
                    for bp in range(NP2 + LAG):
                        gq = g * NP2 + bp
                        if bp < NP2:
                            b0 = bp * 2
                            prefetch_u(gq)
                            us = gq % NUB
                            ha = bp % NHA
                            hbk = HB[bp % 2]
                            for k in range(2):
                                for c in range(8):
                                    PE(lambda e, us=us, c=c, k=k, hbk=hbk, par=par: e.matmul(
                                        out=banks[hbk][:, k * GT:(k + 1) * GT], lhsT=ub[us][:, k, c, :], rhs=n2TC[par][:, c, :],
                                        start=(c == 0), stop=(c == 7)),
                                       r=[("ub", us), ("n2TC", par)], w=[("ps", hbk)])
                            ACT(lambda e, ha=ha, hbk=hbk: e.activation(
                                out=Hg[ha][:], in_=banks[hbk][:].rearrange("p (k t) -> p k t", k=2), func=AF.Gelu),
                                r=[("ps", hbk)], w=[("Hg", ha)])
                            gsrc = Gf[par][:, :, b0:b0 + 2].rearrange("p t k -> p k t")
                            DVE(lambda e, ha=ha, gsrc=gsrc: e.tensor_tensor(out=At[ha][:], in0=Hg[ha][:], in1=gsrc, op=ALU.mult),
                                r=[("Hg", ha), ("Gf", par)], w=[("At", ha)])
                            pend.append((ha, gq, b0))
                        if bp >= LAG:
                            pha, pgq, pb0 = pend.pop(0)
                            prefetch_v(pgq)
                            pvs = pgq % NVB
                            for k in range(2):
                                b = pb0 + k
                                for j in range(NJ):
                                    for n in range(2):
                                        ybk = YB[j][n]
                                        PE(lambda e, pha=pha, pvs=pvs, k=k, j=j, n=n, ybk=ybk, b=b: e.matmul(
                                            out=banks[ybk][:], lhsT=At[pha][:, k, j * P:(j + 1) * P],
                                            rhs=vb[pvs][:, k, n * 512:(n + 1) * 512], start=(b == 0), stop=(b == P - 1)),
                                           r=[("At", pha), ("vb", pvs)], w=[("ps", ybk)])
                            prefetch_v(pgq + NVB - 1)
                        if bp < NP2:
                            prefetch_u(gq + NUB - 1)
                        if bg is not None and bp < NP2:
                            want = ((bp + 1) * nsteps) // NP2
                            while done_steps < want:
                                next(bg, None)
                                done_steps += 1
                    if bg is not None:
                        for _ in bg:
                            pass
                    for j in range(NJ):
                        T = t0 + j
                        DMA(lambda e, T=T: e.dma_start(out=hbC[:], in_=h_scr[T * P:(T + 1) * P, :]),
                            r=["h_scr"], w=["hbC"])
                        for n in range(2):
                            ybk = YB[j][n]
                            DVE(lambda e, n=n, ybk=ybk: e.tensor_tensor(
                                out=hbC[:, n * 512:(n + 1) * 512], in0=banks[ybk][:], in1=hbC[:, n * 512:(n + 1) * 512],
                                op=ALU.add),
                                r=[("ps", ybk), "hbC"], w=["hbC"])
                        ACT(lambda e: e.activation(out=ob[:], in_=hbC[:], func=AF.Square, scale=1.0 / 32.0,
                                                   accum_out=ms3[:, 0:1]),
                            r=["hbC"], w=["ob", "ms3"])
                        ACT(lambda e: e.activation(out=ms3[:, 2:3], in_=ms3[:, 0:1], func=AF.Sqrt, bias=epsT[:, 0:1], scale=1.0),
                            r=["ms3", "epsT"], w=["ms3s"])
                        DVE(lambda e: e.reciprocal(out=ms3[:, 1:2], in_=ms3[:, 2:3]), r=["ms3s"], w=["ms3r"])
                        DVE(lambda e: e.scalar_tensor_tensor(out=ob[:], in0=hbC[:], scalar=ms3[:, 1:2], in1=fgB[:],
                                                             op0=ALU.mult, op1=ALU.mult),
                            r=["hbC", "ms3r", "fgB"], w=["ob"])
                        out_dmas.append(DMA(lambda e, T=T: e.dma_start(out=out[T * P:(T + 1) * P, :], in_=ob[:]),
                                            r=["ob"], w=["out"]))
            sch.barrier()

        sch.barrier()
        sch.final_wait(out_dmas)
        n_wait = sch.lower()
    return nc


def _vecT(v):
    return np.ascontiguousarray(np.asarray(v, np.float32).reshape(8, P).T)


def make_in_maps(inputs, n_cores=8, n_tok=S):
    f = lambda k: np.asarray(inputs[k], np.float32)
    consts = _const_tables()
    vecs = np.stack([_vecT(f("norm1_gain")[0]), _vecT(f("norm2_gain")[0]), _vecT(f("mem_norm_gain")[0]),
                     _vecT(f("pool_scale")[0]), _vecT(f("sgu_ln_gain")[0]), _vecT(f("sgu_ln_bias")[0])], axis=1)
    U = f("peer_u")[0]
    UTh = np.ascontiguousarray(U.reshape(P, P, 8, P).transpose(0, 3, 2, 1)).reshape(P * P, 8 * P)
    wq = f("peer_w_q")[0]
    wqT = np.ascontiguousarray(wq.reshape(D, 16, P).transpose(2, 1, 0))
    keysT = np.ascontiguousarray(np.stack([f("peer_keys1")[0].T, f("peer_keys2")[0].T], axis=1))
    shared = {
        "w_in": np.ascontiguousarray(f("w_in")[0]),
        "vecs": np.ascontiguousarray(vecs),
        "fg": np.ascontiguousarray(f("final_norm_gain").reshape(1, D)),
        "pool_w": np.ascontiguousarray(f("pool_w")[0]),
        "wsT": np.ascontiguousarray(f("sgu_w_s")[0].transpose(2, 0, 1)),
        "bs": np.ascontiguousarray(f("sgu_b_s")[0].reshape(1, 8 * P)),
        "swo": np.ascontiguousarray(f("sgu_w_out")[0]),
        "wkv": np.ascontiguousarray(f("xa_w_kv")[0]),
        "xwo": np.ascontiguousarray(f("xa_w_out")[0]),
        "wout": np.ascontiguousarray(f("w_out")[0]),
        "wqT": wqT,
        "keysT": keysT,
        "UTh": UTh,
        "Vh": np.ascontiguousarray(f("peer_v")[0]),
    }
    shared.update(consts)
    x = f("x")
    mem = f("mem")
    maps = []
    for b in range(n_cores):
        m = dict(shared)
        m["x"] = np.ascontiguousarray(x[b, :n_tok])
        m["mem"] = np.ascontiguousarray(mem[b])
        maps.append(m)
    return maps


def kernel(**inputs):
    nc = build()
    in_maps = make_in_maps(inputs)
    res = run_bass_kernel_spmd(nc, in_maps, core_ids=list(range(8)))
    return np.stack([np.asarray(r["out"], np.float32) for r in res.results], axis=0)
```

```python
import numpy as np
from contextlib import ExitStack
import concourse.bass as bass
import concourse.mybir as mybir
from concourse.bass_utils import run_bass_kernel_spmd

F32 = mybir.dt.float32
BF16 = mybir.dt.bfloat16
U32 = mybir.dt.uint32
AF = mybir.ActivationFunctionType
ALU = mybir.AluOpType
AX = mybir.AxisListType

P = 128
D = 1024
S = 4096
NMEM = 256
INW = 7168
NEXP = 16384
GT = 256
NJ = GT // P
RMS_EPS = 1e-6
LN_EPS = 1e-5
NEG = -1.0e30


class Sched:
    STREAMS = ("pe", "act", "dve", "pool", "sp")

    def __init__(self, nc, es, n_dma_sems=24):
        self.nc = nc
        self.es = es
        self.recs = []
        self.W = {}
        self.R = {}
        self.last = {}
        self.dmas_since_barrier = []
        self.n_dma_sems = n_dma_sems

    def op(self, stream, fn, reads=(), writes=(), dma=False):
        i = len(self.recs)
        deps = {}
        for k in reads:
            for w in self.W.get(k, ()):
                deps[w] = "raw"
        for k in writes:
            rs = self.R.get(k)
            if rs:
                for r in rs:
                    deps.setdefault(r, "war")
                for w in self.W.get(k, ()):
                    deps.setdefault(w, "waw")
                self.W[k] = []
                self.R[k] = []
            else:
                for w in self.W.get(k, ()):
                    deps.setdefault(w, "waw")
        for k in reads:
            self._push(self.R.setdefault(k, []), i, stream, dma)
        for k in writes:
            self._push(self.W.setdefault(k, []), i, stream, dma)
        self.recs.append(dict(stream=stream, fn=fn, dma=dma, deps=deps, need=False, ev=None))
        if fn is not None:
            self.last[stream] = i
        if dma:
            self.dmas_since_barrier.append(i)
        return i

    def _push(self, lst, i, stream, dma):
        if lst and not dma:
            j = lst[-1]
            rj = self.recs[j]
            if rj["stream"] == stream and not rj["dma"]:
                lst[-1] = i
                return
        lst.append(i)

    def barrier(self):
        lasts = dict(self.last)
        dmas = list(self.dmas_since_barrier)
        for s in self.STREAMS:
            deps = {}
            for s2, i in lasts.items():
                if s2 != s or self.recs[i]["dma"]:
                    deps[i] = "raw"
            for i in dmas:
                deps[i] = "raw"
            self.recs.append(dict(stream=s, fn=None, dma=False, deps=deps, need=False, ev=None))
        self.W = {}
        self.R = {}
        self.dmas_since_barrier = []

    def final_wait(self, rec_ids):
        deps = {i: "raw" for i in rec_ids}
        self.recs.append(dict(stream="sp", fn=None, dma=False, deps=deps, need=False, ev=None))

    def lower(self):
        nc = self.nc
        recs = self.recs
        eng = {"pe": nc.tensor, "act": nc.scalar, "dve": nc.vector, "pool": nc.gpsimd, "sp": nc.sync}
        for i, r in enumerate(recs):
            waits = []
            for d, kind in r["deps"].items():
                rd = recs[d]
                if rd["fn"] is None:
                    continue
                if rd["stream"] == r["stream"] and not rd["dma"] and not r["dma"]:
                    if kind != "raw" or r["stream"] == "pe":
                        continue
                waits.append(d)
                rd["need"] = True
            r["waits"] = sorted(waits)
        cnt = {s: 0 for s in self.STREAMS}
        sems = {s: self.es.enter_context(nc.semaphore("sem_" + s)) for s in self.STREAMS}
        semgen = {s: 0 for s in self.STREAMS}
        dsem = [self.es.enter_context(nc.semaphore("dsem%d" % k)) for k in range(self.n_dma_sems)]
        dval = [0] * self.n_dma_sems
        dnext = 0
        seen = {s: {} for s in self.STREAMS}
        n_wait = 0
        for r in recs:
            s = r["stream"]
            e = eng[s]
            sn = seen[s]
            for d in r["waits"]:
                sem, val = recs[d]["ev"]
                if sn.get(id(sem), 0) >= val:
                    continue
                e.wait_ge(sem, val)
                n_wait += 1
                sn[id(sem)] = val
            if r["fn"] is None:
                continue
            if r["dma"]:
                k = dnext
                dnext = (dnext + 1) % self.n_dma_sems
                if dval[k] > 0 and sn.get(id(dsem[k]), 0) < dval[k]:
                    e.wait_ge(dsem[k], dval[k])
                    sn[id(dsem[k])] = dval[k]
                ins = r["fn"](e)
                dval[k] += 16
                ins.then_inc(dsem[k], 16)
                r["ev"] = (dsem[k], dval[k])
            else:
                ins = r["fn"](e)
                if r["need"]:
                    if cnt[s] >= 30000:
                        semgen[s] += 1
                        sems[s] = self.es.enter_context(nc.semaphore("sem_%s_%d" % (s, semgen[s])))
                        cnt[s] = 0
                    cnt[s] += 1
                    ins.then_inc(sems[s], 1)
                    r["ev"] = (sems[s], cnt[s])
        return n_wait


def _const_tables():
    windows = (2, 4, 8, 16)
    s = np.arange(P)[:, None]
    t = np.arange(P)[None, :]
    bands = np.zeros((12, P, P), np.float32)
    for g, w in enumerate(windows):
        cur = ((s <= t) & (s > t - w)).astype(np.float32) / w
        bands[g] = cur - np.eye(P, dtype=np.float32)
        bands[4 + g] = (s > P + t - w).astype(np.float32) / w
        cnt = np.minimum(t + 1, w).astype(np.float32)
        bands[8 + g] = ((s <= t) & (s > t - w)).astype(np.float32) / cnt - np.eye(P, dtype=np.float32)
    consts = {
        "c_ident": np.eye(P, dtype=np.float32),
        "c_bands": np.ascontiguousarray(bands.transpose(1, 0, 2)),
        "c_trilT": (s <= t).astype(np.float32),
        "c_iota": np.tile(np.arange(P, dtype=np.float32)[None, :], (P, 1)),
        "c_iota16": np.tile(np.arange(16, dtype=np.float32)[None, :], (P, 8 * 16)),
    }
    return consts


def build(n_tok=S, debug=False, phases=("A", "B2")):
    nc = bass.Bass("TRN2", target_bir_lowering=False)
    NG = n_tok // GT
    NT = n_tok // P

    def din(name, shape, dt=F32):
        return nc.dram_tensor(name, list(shape), dt, kind="ExternalInput").ap()

    x = din("x", [n_tok, D])
    mem = din("mem", [NMEM, D])
    w_in = din("w_in", [D, INW])
    vecs = din("vecs", [P, 6, 8])
    fg = din("fg", [1, D])
    pool_w = din("pool_w", [4, 256, 256])
    wsT = din("wsT", [P, 8, P])
    bs = din("bs", [1, 8 * P])
    swo = din("swo", [D, D])
    wkv = din("wkv", [D, 2 * D])
    xwo = din("xwo", [D, D])
    wout = din("wout", [D, D])
    wqT = din("wqT", [P, 16, D])
    keysT = din("keysT", [P, 2, P])
    UTh = din("UTh", [P * P, 8 * P])
    Vh = din("Vh", [NEXP, D])
    c_ident = din("c_ident", [P, P])
    c_bands = din("c_bands", [P, 12, P])
    c_trilT = din("c_trilT", [P, P])
    c_iota = din("c_iota", [P, P])
    c_iota16 = din("c_iota16", [P, 8 * 16 * 16])

    out = nc.dram_tensor("out", [n_tok, D], F32, kind="ExternalOutput").ap()
    kind_dbg = "ExternalOutput" if debug else "Internal"
    h_scr = nc.dram_tensor("h_scr", [n_tok, D], F32, kind=kind_dbg).ap()
    jt_scr = nc.dram_tensor("jt_scr", [NT * P, 3 * P], F32, kind=kind_dbg).ap()
    n2t_scr = nc.dram_tensor("n2t_scr", [NG * P, 8 * GT], BF16, kind="Internal").ap()
    NWCOL = INW + 3 * D + 2048
    win_bf = nc.dram_tensor("win_bf", [D, NWCOL], BF16, kind="Internal").ap()
    ut_bf = nc.dram_tensor("ut_bf", [P * P, 8 * P], BF16, kind="Internal").ap()
    v_bf = nc.dram_tensor("v_bf", [NEXP, D], BF16, kind="Internal").ap()

    es_all = ExitStack()
    sch = Sched(nc, es_all)

    def PE(fn, r=(), w=()):
        return sch.op("pe", fn, r, w)

    def ACT(fn, r=(), w=()):
        return sch.op("act", fn, r, w)

    def DVE(fn, r=(), w=()):
        return sch.op("dve", fn, r, w)

    def POOL(fn, r=(), w=()):
        return sch.op("pool", fn, r, w)

    def DMA(fn, r=(), w=(), q="sp"):
        return sch.op(q, fn, r, w, dma=True)

    out_dmas = []

    with es_all:
        es = es_all
        sb = lambda name, shape, dt: es.enter_context(nc.sbuf_tensor(name, list(shape), dt))
        banks = [es.enter_context(nc.psum_tensor("bank%d" % i, [P, 512], F32)) for i in range(8)]
        bank_rr = [0]

        def next_bank(pool=range(8)):
            pool = list(pool)
            b = pool[bank_rr[0] % len(pool)]
            bank_rr[0] += 1
            return b

        ident = sb("ident", [P, P], BF16)
        vec_t = sb("vec_t", [P, 6, 8], F32)
        fgB = sb("fgB", [P, D], F32)
        epsT = sb("epsT", [P, 2], F32)
        POOL(lambda e: e.memset(epsT[:, 0:1], RMS_EPS), w=["epsT"])
        POOL(lambda e: e.memset(epsT[:, 1:2], LN_EPS), w=["epsT"])
        DMA(lambda e: e.dma_start(out=ident[:], in_=c_ident[:, :]), w=["ident"], q="pool")
        ident_f = sb("ident_f", [P, P], F32)
        DMA(lambda e: e.dma_start(out=ident_f[:], in_=c_ident[:, :]), w=["ident_f"])
        DMA(lambda e: e.dma_start(out=vec_t[:], in_=vecs[:, :, :]), w=["vec"])
        DMA(lambda e: e.dma_start(out=fgB[:], in_=fg.partition_broadcast(P)), w=["fgB"])
        G1, G2, GM, PSC, LNG, LNB = range(6)

        for c in range(8):
            DMA(lambda e, c=c: e.dma_start(out=win_bf[c * P:(c + 1) * P, 0:INW], in_=w_in[c * P:(c + 1) * P, :]),
                w=["win_bf"], q="pool")
        for wi, wsrc in enumerate((swo, xwo, wout)):
            for c in range(0, 8, 2):
                DMA(lambda e, c=c, wi=wi, wsrc=wsrc: e.dma_start(
                    out=win_bf[c * P:(c + 2) * P, INW + wi * D:INW + (wi + 1) * D], in_=wsrc[c * P:(c + 2) * P, :]),
                    w=["win_bf"], q="pool")

        def cast_experts(b_lo=0, b_hi=P, dep=()):
            for b in range(b_lo, b_hi):
                DMA(lambda e, b=b: e.dma_start(out=ut_bf[b * P:(b + 1) * P, :], in_=UTh[b * P:(b + 1) * P, :]),
                    r=list(dep), w=["ut_bf"], q="pool")
                DMA(lambda e, b=b: e.dma_start(out=v_bf[b * P:(b + 1) * P, :], in_=Vh[b * P:(b + 1) * P, :]),
                    r=list(dep), w=["v_bf"], q="pool")

        def rms_tile(src, srckey, xn, xnkey, ms, mskey):
            ACT(lambda e: e.activation(out=xn[:], in_=src[:], func=AF.Square, scale=1.0 / 32.0,
                                       accum_out=ms[:, 0:1]),
                r=[srckey], w=[xnkey, mskey])
            ACT(lambda e: e.activation(out=ms[:, 1:2], in_=ms[:, 0:1], func=AF.Sqrt, bias=epsT[:, 0:1], scale=1.0),
                r=[mskey, "epsT"], w=[mskey + "s"])
            DVE(lambda e: e.reciprocal(out=ms[:, 2:3], in_=ms[:, 1:2]), r=[mskey + "s"], w=[mskey + "r"])
            ACT(lambda e: e.activation(out=xn[:], in_=src[:], func=AF.Copy, scale=ms[:, 2:3]),
                r=[srckey, mskey + "r"], w=[xnkey])

        def transpose_tile(xn, xnkey, bA, bB, j, ncol):
            for c in range(8):
                bk = bA if c < 4 else bB
                psb = banks[bk][:].bitcast(BF16)
                o = (c % 4) * ncol + j * P
                PE(lambda e, c=c, psb=psb, o=o: e.transpose(out=psb[:, o:o + P], in_=xn[:, c * P:(c + 1) * P],
                                                           identity=ident[:]),
                   r=[xnkey, "ident"], w=[("ps", bk)])

        def evac_T(dst, dstkey, bA, bB, gidx, ncol):
            for k, bk in enumerate((bA, bB)):
                psb = banks[bk][:].bitcast(BF16)
                DVE(lambda e, k=k, psb=psb: e.tensor_tensor(
                    out=dst[:, 4 * k:4 * k + 4, :],
                    in0=psb[:, 0:4 * ncol].rearrange("p (c t) -> p c t", c=4),
                    in1=vec_t[:, gidx, 4 * k:4 * k + 4].unsqueeze(2).to_broadcast([P, 4, ncol]),
                    op=ALU.mult),
                    r=[("ps", bk), "vec"], w=[dstkey])

        if "A" in phases:
            esA = ExitStack()
            with esA:
                sbA = lambda name, shape, dt: esA.enter_context(nc.sbuf_tensor(name, list(shape), dt))
                bands = sbA("bands", [P, 12, P], BF16)
                WmT = sbA("WmT", [P, 8, P], BF16)
                Cm = sbA("Cm", [P, 8, P], F32)
                pw_t = sbA("pw_t", [P, 8, 256], BF16)
                kT = sbA("kT", [P, 8, NMEM], BF16)
                vmem = sbA("vmem", [P, 2, D], BF16)
                DMA(lambda e: e.dma_start(out=bands[:], in_=c_bands[:, :, :]), w=["bands"], q="pool")
                DMA(lambda e: e.dma_start(out=pw_t[:], in_=pool_w.rearrange("g (k p) d -> p (g k) d", p=P)), w=["pw"], q="pool")

                esS = ExitStack()
                with esS:
                    sbS = lambda name, shape, dt: esS.enter_context(nc.sbuf_tensor(name, list(shape), dt))
                    wsT_f = sbS("wsT_f", [P, 8, P], F32)
                    tril_f = sbS("tril_f", [P, P], F32)
                    BSb = sbS("BSb", [P, 8, P], F32)
                    ones_bf = sbS("ones_bf", [P, P], BF16)
                    wkv_t = sbS("wkv_t", [P, 8, 2 * D], BF16)
                    memf = [sbS("memf%d" % i, [P, D], F32) for i in range(2)]
                    memn = [sbS("memn%d" % i, [P, D], BF16) for i in range(2)]
                    mms = [sbS("mms%d" % i, [P, 4], F32) for i in range(2)]
                    memnT = sbS("memnT", [P, 8, NMEM], BF16)

                    DMA(lambda e: e.dma_start(out=wsT_f[:], in_=wsT[:, :, :]), w=["wsT_f"])
                    DMA(lambda e: e.dma_start(out=tril_f[:], in_=c_trilT[:, :]), w=["tril_f"])
                    DMA(lambda e: e.dma_start(out=BSb[:].rearrange("p h t -> p (h t)"), in_=bs.partition_broadcast(P)), w=["BSb"])
                    DMA(lambda e: e.dma_start(out=wkv_t[:], in_=wkv.rearrange("(c p) n -> p c n", p=P)), w=["wkv"], q="pool")
                    POOL(lambda e: e.memset(ones_bf[:], 1.0), w=["ones"])
                    DVE(lambda e: e.tensor_tensor(out=WmT[:], in0=wsT_f[:],
                                                  in1=tril_f[:].unsqueeze(1).to_broadcast([P, 8, P]), op=ALU.mult),
                        r=["wsT_f", "tril_f"], w=["WmT"])
                    for half in range(2):
                        bk = next_bank()
                        for hh in range(4):
                            h = half * 4 + hh
                            PE(lambda e, h=h, hh=hh, bk=bk: e.matmul(out=banks[bk][:, hh * P:(hh + 1) * P], lhsT=ones_bf[:],
                                                                     rhs=WmT[:, h, :], start=True, stop=True),
                               r=["ones", "WmT"], w=[("ps", bk)])
                        for hh in range(4):
                            h = half * 4 + hh
                            DVE(lambda e, h=h, hh=hh, bk=bk: e.scalar_tensor_tensor(
                                out=Cm[:, h, :], in0=banks[bk][:, hh * P:(hh + 1) * P], scalar=vec_t[:, LNB, h:h + 1],
                                in1=BSb[:, h, :], op0=ALU.mult, op1=ALU.add),
                                r=[("ps", bk), "vec", "BSb"], w=["Cm"])
                    bA, bB = next_bank(), next_bank()
                    for mt in range(2):
                        DMA(lambda e, mt=mt: e.dma_start(out=memf[mt][:], in_=mem[mt * P:(mt + 1) * P, :]), w=[("memf", mt)])
                        rms_tile(memf[mt], ("memf", mt), memn[mt], ("memn", mt), mms[mt], "mms%d" % mt)
                        transpose_tile(memn[mt], ("memn", mt), bA, bB, mt, NMEM)
                    evac_T(memnT, "memnT", bA, bB, GM, NMEM)
                    for hc in range(8):
                        if hc % 2 == 0:
                            bk = next_bank()
                        for c in range(8):
                            PE(lambda e, hc=hc, c=c, bk=bk: e.matmul(
                                out=banks[bk][:, (hc % 2) * NMEM:(hc % 2 + 1) * NMEM],
                                lhsT=wkv_t[:, c, hc * P:(hc + 1) * P], rhs=memnT[:, c, :], start=(c == 0), stop=(c == 7)),
                               r=["wkv", "memnT"], w=[("ps", bk)])
                        if hc % 2 == 1:
                            ACT(lambda e, hc=hc, bk=bk: e.activation(
                                out=kT[:, hc - 1:hc + 1, :], in_=banks[bk][:].rearrange("p (a m) -> p a m", a=2), func=AF.Copy),
                                r=[("ps", bk)], w=["kT"])
                    for mt in range(2):
                        for n in range(2):
                            bk = next_bank()
                            for c in range(8):
                                PE(lambda e, mt=mt, n=n, c=c, bk=bk: e.matmul(
                                    out=banks[bk][:], lhsT=memnT[:, c, mt * P:(mt + 1) * P],
                                    rhs=wkv_t[:, c, D + n * 512:D + (n + 1) * 512], start=(c == 0), stop=(c == 7)),
                                   r=["wkv", "memnT"], w=[("ps", bk)])
                            ACT(lambda e, mt=mt, n=n, bk=bk: e.activation(out=vmem[:, mt, n * 512:(n + 1) * 512],
                                                                         in_=banks[bk][:], func=AF.Copy),
                                r=[("ps", bk)], w=["vmem"])
                    wqT_t = sbS("wqT_t", [P, 16, D], BF16)
                    keysT_t = sbS("keysT_t", [P, 2, P], BF16)
                    Ws_t = sbS("Ws_t", [P, 8, 2048], BF16)
                    DMA(lambda e: e.dma_start(out=wqT_t[:], in_=wqT[:, :, :]), w=["wqT"], q="pool")
                    DMA(lambda e: e.dma_start(out=keysT_t[:], in_=keysT[:, :, :]), w=["keysT"], q="pool")
                    for dc in range(8):
                        for ib in range(4):
                            bk = next_bank()
                            for ii in range(4):
                                i = ib * 4 + ii
                                PE(lambda e, dc=dc, i=i, ii=ii, bk=bk: e.matmul(
                                    out=banks[bk][:, ii * P:(ii + 1) * P], lhsT=wqT_t[:, i, dc * P:(dc + 1) * P],
                                    rhs=keysT_t[:, i % 2, :], start=True, stop=True),
                                   r=["wqT", "keysT"], w=[("ps", bk)])
                            ACT(lambda e, dc=dc, ib=ib, bk=bk: e.activation(out=Ws_t[:, dc, ib * 512:(ib + 1) * 512],
                                                                          in_=banks[bk][:], func=AF.Copy),
                                r=[("ps", bk)], w=["Ws_t"])
                    DMA(lambda e: e.dma_start(out=win_bf[:, INW + 3 * D:NWCOL].rearrange("(c p) n -> p c n", p=P), in_=Ws_t[:]),
                        r=["Ws_t"], w=["win_bf"])
                sch.barrier()

                xbuf = [sbA("xbuf%d" % i, [P, D], F32) for i in range(2)]
                xn = [sbA("xn%d" % i, [P, D], BF16) for i in range(2)]
                ms = [sbA("ms%d" % i, [P, 4], F32) for i in range(2)]
                nT = sbA("nT", [P, 8, GT], BF16)
                NWB = 3
                wb = [sbA("wb%d" % i, [P, 8, 512], BF16) for i in range(NWB)]
                Pr = [sbA("Pr%d" % i, [P, D], BF16) for i in range(4)]
                gv = [sbA("gv%d" % i, [P, D], F32) for i in range(2)]
                lst = [sbA("lst%d" % i, [P, 8], F32) for i in range(2)]
                zt = [sbA("zt%d" % i, [P, D], BF16) for i in range(2)]
                uT = sbA("uT", [P, 8, GT], BF16)
                qT = sbA("qT", [P, 8, GT], BF16)
                gT = sbA("gT", [P, 24, GT], BF16)
                diffT = sbA("diffT", [P, 8, GT], BF16)
                gatedT = sbA("gatedT", [P, 8, GT], BF16)
                tmpS = sbA("tmpS", [P, 4, P], F32)
                smx = [sbA("smx%d" % i, [P, 16], F32) for i in range(2)]
                ex = [sbA("ex%d" % i, [P, 4, NMEM], BF16) for i in range(2)]
                probsT = sbA("probsT", [P, 8, GT], BF16)
                oT = sbA("oT", [P, 8, GT], BF16)
                M32 = sbA("M32", [P, 8, GT], F32)
                tmpM = sbA("tmpM", [P, 2, GT], F32)
                Mb = sbA("Mb", [P, 8, GT], BF16)
                pace = sbA("pace", [P, 2], F32)
                iota16 = sbA("iota16", [P, 16], F32)
                DMA(lambda e: e.dma_start(out=iota16[:], in_=c_iota[:, 0:16]), w=["iota16"])
                n2T = sbA("n2T", [P, 8, GT], BF16)
                Sb = [sbA("Sb%d" % i, [P, 2048], F32) for i in range(4)]
                S2x = sbA("S2x", [P, 2048], F32)
                Vt = [sbA("Vt%d" % i, [P, 16, 16], F32) for i in range(2)]
                It = [sbA("It%d" % i, [P, 16, 16], U32) for i in range(2)]
                Itf = [sbA("Itf%d" % i, [P, 16, 16], F32) for i in range(2)]
                Bt = [sbA("Bt%d" % i, [P, 8, 16], F32) for i in range(2)]
                CI = [sbA("CI%d" % i, [P, 8, 16], U32) for i in range(2)]
                CIf = [sbA("CIf%d" % i, [P, 3, 128], F32) for i in range(2)]
                CIu = [sbA("CIu%d" % i, [P, 2, 128], U32) for i in range(2)]
                exg = [sbA("exg%d" % i, [P, 128], F32) for i in range(2)]
                zs = [sbA("zs%d" % i, [P, 16], F32) for i in range(2)]
                JB = [sbA("JB%d" % i, [P, 3, 128], F32) for i in range(2)]
                JT = [sbA("JT%d" % i, [P, 3, 128], F32) for i in range(2)]
                n2t_v = n2t_scr.rearrange("(g p) f -> g p f", p=P)
                NWB_G = 24

                win_v = win_bf.rearrange("(c p) n -> p c n", p=P)
                wb_ctr = [0]

                def load_wb(gb):
                    while wb_ctr[0] <= min(gb + NWB - 1, NG * NWB_G - 1):
                        idx = wb_ctr[0]
                        wb_ctr[0] += 1
                        DMA(lambda e, n=idx % NWB_G, slot=idx % NWB: e.dma_start(out=wb[slot][:], in_=win_v[:, :, n * 512:(n + 1) * 512]),
                            r=["win_bf"], w=[("wb", idx % NWB)])
                    return gb % NWB

                bgq = []

                def tick(k):
                    n = 0
                    while bgq and (k is None or n < k):
                        try:
                            next(bgq[0])
                            n += 1
                        except StopIteration:
                            bgq.pop(0)

                def routing_gen(t0, par):
                    for j in range(NJ):
                        T = t0 + j
                        sj = par * 2 + j
                        Sbj = Sb[sj]
                        SbK = [("Sb", sj, n) for n in range(4)]
                        for i0 in range(0, 16, 4):
                            for i in range(i0, i0 + 4):
                                DVE(lambda e, i=i, j=j, sj=sj: e.max(out=Vt[j][:, i, 0:8], in_=Sb[sj][:, i * P:(i + 1) * P]),
                                    r=[("Sb", sj, i // 4)], w=[("Vt", j, i, 0)])
                            yield
                        for i0 in range(0, 16, 4):
                            for i in range(i0, i0 + 4):
                                DVE(lambda e, i=i, j=j, sj=sj: e.max_index(out=It[j][:, i, 0:8], in_max=Vt[j][:, i, 0:8],
                                                                    in_values=Sb[sj][:, i * P:(i + 1) * P]),
                                    r=[("Sb", sj, i // 4), ("Vt", j, i, 0)], w=[("It", j, i, 0)])
                            yield
                        for i0 in range(0, 16, 4):
                            for i in range(i0, i0 + 4):
                                DVE(lambda e, i=i, j=j, sj=sj: e.match_replace(out=S2x[:, i * P:(i + 1) * P], in_to_replace=Vt[j][:, i, 0:8],
                                                                        in_values=Sb[sj][:, i * P:(i + 1) * P], imm_value=NEG),
                                    r=[("Sb", sj, i // 4), ("Vt", j, i, 0)], w=[("S2", i)])
                            yield
                        for i0 in range(0, 16, 4):
                            for i in range(i0, i0 + 4):
                                DVE(lambda e, i=i, j=j, sj=sj: e.max(out=Vt[j][:, i, 8:16], in_=S2x[:, i * P:(i + 1) * P]),
                                    r=[("S2", i)], w=[("Vt", j, i, 1)])
                            yield
                        for i0 in range(0, 16, 4):
                            for i in range(i0, i0 + 4):
                                DVE(lambda e, i=i, j=j, sj=sj: e.max_index(out=It[j][:, i, 8:16], in_max=Vt[j][:, i, 8:16],
                                                                    in_values=S2x[:, i * P:(i + 1) * P]),
                                    r=[("S2", i), ("Vt", j, i, 1)], w=[("It", j, i, 1)])
                            yield
                        allV = [("Vt", j, i, k) for i in range(16) for k in range(2)]
                        allI = [("It", j, i, k) for i in range(16) for k in range(2)]
                        DVE(lambda e, j=j: e.tensor_copy(out=Itf[j][:], in_=It[j][:]), r=allI, w=[("Itf", j)])
                        cand = Sbj[:].rearrange("p (h c) -> p h c", h=8)
                        cand2 = S2x[:].rearrange("p (h c) -> p h c", h=8)
                        Vv = Vt[j][:].rearrange("p (h f) k -> p h f k", f=2)
                        DVE(lambda e, cand=cand, Vv=Vv: e.tensor_tensor(
                            out=cand.rearrange("p h (a b) -> p h a b", a=16),
                            in0=Vv[:, :, 0, :].unsqueeze(3).to_broadcast([P, 8, 16, 16]),
                            in1=Vv[:, :, 1, :].unsqueeze(2).to_broadcast([P, 8, 16, 16]), op=ALU.add),
                            r=allV + SbK, w=SbK)
                        yield
                        for h0 in range(0, 8, 4):
                            for h in range(h0, h0 + 4):
                                DVE(lambda e, h=h, j=j, cand=cand: e.max(out=Bt[j][:, h, 0:8], in_=cand[:, h, :]),
                                    r=SbK, w=[("Bt", j, h, 0)])
                            yield
                        for h0 in range(0, 8, 4):
                            for h in range(h0, h0 + 4):
                                DVE(lambda e, h=h, j=j, cand=cand: e.max_index(out=CI[j][:, h, 0:8], in_max=Bt[j][:, h, 0:8],
                                                                               in_values=cand[:, h, :]),
                                    r=SbK + [("Bt", j, h, 0)], w=[("CI", j, h, 0)])
                            yield
                        for h0 in range(0, 8, 4):
                            for h in range(h0, h0 + 4):
                                DVE(lambda e, h=h, j=j, cand=cand, cand2=cand2: e.match_replace(
                                    out=cand2[:, h, :], in_to_replace=Bt[j][:, h, 0:8], in_values=cand[:, h, :], imm_value=NEG),
                                    r=SbK + [("Bt", j, h, 0)], w=[("S2", 2 * h), ("S2", 2 * h + 1)])
                            yield
                        for h0 in range(0, 8, 4):
                            for h in range(h0, h0 + 4):
                                DVE(lambda e, h=h, j=j, cand2=cand2: e.max(out=Bt[j][:, h, 8:16], in_=cand2[:, h, :]),
                                    r=[("S2", 2 * h), ("S2", 2 * h + 1)], w=[("Bt", j, h, 1)])
                            yield
                        for h0 in range(0, 8, 4):
                            for h in range(h0, h0 + 4):
                                DVE(lambda e, h=h, j=j, cand2=cand2: e.max_index(out=CI[j][:, h, 8:16], in_max=Bt[j][:, h, 8:16],
                                                                                in_values=cand2[:, h, :]),
                                    r=[("S2", 2 * h), ("S2", 2 * h + 1), ("Bt", j, h, 1)], w=[("CI", j, h, 1)])
                            yield
                        allB = [("Bt", j, h, k) for h in range(8) for k in range(2)]
                        allC = [("CI", j, h, k) for h in range(8) for k in range(2)]
                        POOL(lambda e, j=j: e.tensor_tensor(
                            out=exg[j][:].rearrange("p (h k) -> p h k", h=8), in0=Bt[j][:],
                            in1=Bt[j][:, :, 0:1].to_broadcast([P, 8, 16]), op=ALU.subtract),
                            r=allB, w=[("exg", j)])
                        ACT(lambda e, j=j: e.activation(out=exg[j][:], in_=exg[j][:], func=AF.Exp),
                            r=[("exg", j)], w=[("exg", j)])
                        DVE(lambda e, j=j: e.tensor_reduce(out=zs[j][:, 0:8], in_=exg[j][:].rearrange("p (h k) -> p h k", h=8),
                                                           axis=AX.X, op=ALU.add),
                            r=[("exg", j)], w=[("zs", j)])
                        DVE(lambda e, j=j: e.reciprocal(out=zs[j][:, 8:16], in_=zs[j][:, 0:8]), r=[("zs", j)], w=[("zr", j)])
                        POOL(lambda e, j=j: e.tensor_tensor(
                            out=JB[j][:, 2, :].rearrange("p (h k) -> p h k", h=8),
                            in0=exg[j][:].rearrange("p (h k) -> p h k", h=8),
                            in1=zs[j][:, 8:16].unsqueeze(2).to_broadcast([P, 8, 16]), op=ALU.mult),
                            r=[("exg", j), ("zr", j)], w=[("JB", j, 2)])
                        yield
                        DVE(lambda e, j=j: e.tensor_single_scalar(out=CIu[j][:, 0, :], in_=CI[j][:].rearrange("p h k -> p (h k)"),
                                                                  scalar=15, op=ALU.bitwise_and),
                            r=allC, w=[("CIu", j, 0)])
                        DVE(lambda e, j=j: e.tensor_single_scalar(out=CIu[j][:, 1, :], in_=CI[j][:].rearrange("p h k -> p (h k)"),
                                                                  scalar=4, op=ALU.logical_shift_right),
                            r=allC, w=[("CIu", j, 1)])
                        DVE(lambda e, j=j: e.tensor_copy(out=CIf[j][:, 1, :], in_=CIu[j][:, 0, :]),
                            r=[("CIu", j, 0)], w=[("CIf", j, 1)])
                        DVE(lambda e, j=j: e.tensor_copy(out=CIf[j][:, 2, :], in_=CIu[j][:, 1, :]),
                            r=[("CIu", j, 1)], w=[("CIf", j, 2)])
                        yield
                        Iv = Itf[j][:].rearrange("p (h f) k -> p h f k", f=2)
                        specs = ((0, 2, 0, Sbj, SbK), (1, 1, 1, S2x, [("S2", i) for i in range(16)]))
                        for which, row, half, mbuf, mkeys in specs:
                            mk = mbuf[:].rearrange("p (h q a) -> p h q a", h=8, q=16)
                            DVE(lambda e, j=j, row=row, mk=mk: e.tensor_tensor(
                                out=mk, in0=iota16[:].unsqueeze(1).unsqueeze(1).to_broadcast([P, 8, 16, 16]),
                                in1=CIf[j][:, row, :].rearrange("p (h q) -> p h q", h=8).unsqueeze(3).to_broadcast([P, 8, 16, 16]),
                                op=ALU.is_equal),
                                r=[("CIf", j, row), "iota16"] + mkeys, w=mkeys)
                            POOL(lambda e, half=half, mk=mk, Iv=Iv: e.tensor_tensor(
                                out=mk, in0=mk, in1=Iv[:, :, half, :].unsqueeze(2).to_broadcast([P, 8, 16, 16]), op=ALU.mult),
                                r=mkeys + [("Itf", j)], w=mkeys)
                            yield
                        yield
                        for which, row, half, mbuf, mkeys in specs:
                            mk = mbuf[:].rearrange("p (h q a) -> p h q a", h=8, q=16)
                            DVE(lambda e, j=j, which=which, mk=mk: e.tensor_reduce(
                                out=JB[j][:, which, :].rearrange("p (h q) -> p h q", h=8), in_=mk, axis=AX.X, op=ALU.add),
                                r=mkeys, w=[("JB", j, which)])
                        yield
                        bk = next_bank()
                        for w3 in range(3):
                            PE(lambda e, w3=w3, j=j, bk=bk: e.transpose(out=banks[bk][:, w3 * P:(w3 + 1) * P], in_=JB[j][:, w3, :],
                                                                       identity=ident_f[:]),
                               r=[("JB", j, w3), "ident_f"], w=[("ps", bk)])
                        ACT(lambda e, j=j, bk=bk: e.activation(out=JT[j][:].rearrange("p w t -> p (w t)"), in_=banks[bk][:, 0:3 * P],
                                                               func=AF.Copy),
                            r=[("ps", bk)], w=[("JT", j)])
                        DMA(lambda e, T=T, j=j: e.dma_start(out=jt_scr[T * P:(T + 1) * P, :], in_=JT[j][:].rearrange("p w t -> p (w t)")),
                            r=[("JT", j)], w=["jt_scr"])
                        yield

                xslot = [0]
                for g in range(NG):
                    t0 = g * NJ
                    bA, bB = next_bank(), next_bank()
                    for j in range(NJ):
                        T = t0 + j
                        sl = xslot[0] % 2
                        xslot[0] += 1
                        DMA(lambda e, T=T, sl=sl: e.dma_start(out=xbuf[sl][:], in_=x[T * P:(T + 1) * P, :]), w=[("xbuf", sl)])
                        tick(3)
                        rms_tile(xbuf[sl], ("xbuf", sl), xn[sl], ("xn", sl), ms[sl], "ms%d" % sl)
                        transpose_tile(xn[sl], ("xn", sl), bA, bB, j, GT)
                    tick(3)
                    evac_T(nT, "nT", bA, bB, G1, GT)
                    DVE(lambda e: e.tensor_copy(out=pace[:, 0:1], in_=epsT[:, 0:1]), r=["epsT"], w=[("pace", g)])
                    nb_ = P // NG
                    cast_experts(g * nb_, (g + 1) * nb_, dep=[("pace", g)])

                    for n in range(14):
                        slot = load_wb(g * NWB_G + n)
                        tick(4)
                        if n in (0, 1, 4, 5):
                            for j in range(NJ):
                                T = t0 + j
                                bk = next_bank()
                                for c in range(8):
                                    PE(lambda e, c=c, j=j, bk=bk, slot=slot: e.matmul(
                                        out=banks[bk][:], lhsT=nT[:, c, j * P:(j + 1) * P], rhs=wb[slot][:, c, :],
                                        start=(c == 0), stop=(c == 7)),
                                       r=["nT", ("wb", slot)], w=[("ps", bk)])
                                if n < 2:
                                    ACT(lambda e, T=T, n=n, bk=bk: e.activation(
                                        out=Pr[T % 4][:, n * 512:(n + 1) * 512], in_=banks[bk][:], func=AF.Copy),
                                        r=[("ps", bk)], w=[("Pr", T % 4)])
                                else:
                                    nn = n - 4
                                    ACT(lambda e, j=j, nn=nn, bk=bk: e.activation(
                                        out=gv[j][:, nn * 512:(nn + 1) * 512], in_=banks[bk][:], func=AF.Gelu,
                                        accum_out=lst[j][:, nn:nn + 1]),
                                        r=[("ps", bk)], w=[("gv", j), ("lst", j, nn)])
                        else:
                            for kk in range(2):
                                bk = next_bank()
                                for k2 in range(2):
                                    k = kk * 2 + k2
                                    for c in range(8):
                                        PE(lambda e, c=c, k=k, k2=k2, bk=bk, slot=slot: e.matmul(
                                            out=banks[bk][:, k2 * GT:(k2 + 1) * GT], lhsT=wb[slot][:, c, k * P:(k + 1) * P],
                                            rhs=nT[:, c, :], start=(c == 0), stop=(c == 7)),
                                           r=["nT", ("wb", slot)], w=[("ps", bk)])
                                src = banks[bk][:].rearrange("p (a t) -> p a t", a=2)
                                if n in (2, 3):
                                    ch = (n - 2) * 4 + kk * 2
                                    ACT(lambda e, ch=ch, src=src: e.activation(out=uT[:, ch:ch + 2, :], in_=src, func=AF.Gelu),
                                        r=[("ps", bk)], w=["uT"])
                                elif n in (6, 7):
                                    ch = (n - 6) * 4 + kk * 2
                                    ACT(lambda e, ch=ch, src=src: e.activation(out=qT[:, ch:ch + 2, :], in_=src, func=AF.Copy),
                                        r=[("ps", bk)], w=["qT"])
                                else:
                                    ch = (n - 8) * 4 + kk * 2
                                    ACT(lambda e, ch=ch, src=src: e.activation(out=gT[:, ch:ch + 2, :], in_=src, func=AF.Sigmoid),
                                        r=[("ps", bk)], w=["gT"])
                        if n == 5:
                            for j in range(NJ):
                                ACT(lambda e, j=j: e.activation(out=zt[j][:], in_=gv[j][:], func=AF.Square,
                                                                accum_out=lst[j][:, 2:3]),
                                    r=[("gv", j)], w=[("zt", j), ("lst", j, 2)])
                                tick(1)
                                DVE(lambda e, j=j: e.tensor_scalar(out=lst[j][:, 3:4], in0=lst[j][:, 0:1], scalar1=lst[j][:, 1:2],
                                                                   scalar2=1.0 / D, op0=ALU.add, op1=ALU.mult),
                                    r=[("lst", j, 0), ("lst", j, 1)], w=[("lst", j, 3)])
                                DVE(lambda e, j=j: e.tensor_tensor(out=lst[j][:, 4:5], in0=lst[j][:, 3:4], in1=lst[j][:, 3:4],
                                                                   op=ALU.mult),
                                    r=[("lst", j, 3)], w=[("lst", j, 4)])
                                DVE(lambda e, j=j: e.scalar_tensor_tensor(out=lst[j][:, 5:6], in0=lst[j][:, 2:3], scalar=1.0 / D,
                                                                          in1=lst[j][:, 4:5], op0=ALU.mult, op1=ALU.subtract),
                                    r=[("lst", j, 2), ("lst", j, 4)], w=[("lst", j, 5)])
                                ACT(lambda e, j=j: e.activation(out=lst[j][:, 7:8], in_=lst[j][:, 5:6], func=AF.Sqrt,
                                                                bias=epsT[:, 1:2], scale=1.0),
                                    r=[("lst", j, 5), "epsT"], w=[("lst", j, 7)])
                                DVE(lambda e, j=j: e.reciprocal(out=lst[j][:, 6:7], in_=lst[j][:, 7:8]),
                                    r=[("lst", j, 7)], w=[("lst", j, 6)])
                                DVE(lambda e, j=j: e.tensor_scalar(out=zt[j][:], in0=gv[j][:], scalar1=lst[j][:, 3:4],
                                                                   scalar2=lst[j][:, 6:7], op0=ALU.subtract, op1=ALU.mult),
                                    r=[("gv", j), ("lst", j, 3), ("lst", j, 6)], w=[("zt", j)])

                    for j in range(NJ):
                        T = t0 + j
                        for half in range(2):
                            bk = next_bank()
                            for cc in range(4):
                                c = half * 4 + cc
                                gi = c // 2
                                band0 = (8 + gi) if T == 0 else gi
                                PE(lambda e, c=c, cc=cc, T=T, bk=bk, band0=band0: e.matmul(
                                    out=banks[bk][:, cc * P:(cc + 1) * P], lhsT=Pr[T % 4][:, c * P:(c + 1) * P],
                                    rhs=bands[:, band0, :], start=True, stop=(T == 0)),
                                   r=[("Pr", T % 4), "bands"], w=[("ps", bk)])
                                if T > 0:
                                    PE(lambda e, c=c, cc=cc, T=T, bk=bk, gi=gi: e.matmul(
                                        out=banks[bk][:, cc * P:(cc + 1) * P], lhsT=Pr[(T - 1) % 4][:, c * P:(c + 1) * P],
                                        rhs=bands[:, 4 + gi, :], start=False, stop=True),
                                       r=[("Pr", (T - 1) % 4), "bands"], w=[("ps", bk)])
                            ACT(lambda e, half=half, j=j, bk=bk: e.activation(
                                out=diffT[:, half * 4:half * 4 + 4, j * P:(j + 1) * P],
                                in_=banks[bk][:].rearrange("p (c t) -> p c t", c=4), func=AF.Copy),
                                r=[("ps", bk)], w=["diffT"])
                    for dc in range(8):
                        if dc % 2 == 0:
                            bk = next_bank()
                        gi = dc // 2
                        for kc in range(2):
                            PE(lambda e, dc=dc, gi=gi, kc=kc, bk=bk: e.matmul(
                                out=banks[bk][:, (dc % 2) * GT:(dc % 2 + 1) * GT],
                                lhsT=pw_t[:, gi * 2 + kc, (dc % 2) * P:(dc % 2 + 1) * P], rhs=diffT[:, gi * 2 + kc, :],
                                start=(kc == 0), stop=(kc == 1)),
                               r=["pw", "diffT"], w=[("ps", bk)])
                        tick(1)
                        DVE(lambda e, dc=dc, bk=bk: e.scalar_tensor_tensor(
                            out=M32[:, dc, :], in0=banks[bk][:, (dc % 2) * GT:(dc % 2 + 1) * GT],
                            scalar=vec_t[:, PSC, dc:dc + 1], in1=gT[:, dc, :], op0=ALU.mult, op1=ALU.mult),
                            r=[("ps", bk), "vec", "gT"], w=["M32"])

                    for j in range(NJ):
                        for half in range(2):
                            bk = next_bank()
                            for hh in range(4):
                                h = half * 4 + hh
                                PE(lambda e, h=h, hh=hh, j=j, bk=bk: e.matmul(
                                    out=banks[bk][:, hh * P:(hh + 1) * P], lhsT=zt[j][:, h * P:(h + 1) * P],
                                    rhs=WmT[:, h, :], start=True, stop=True),
                                   r=[("zt", j), "WmT"], w=[("ps", bk)])
                            h0 = half * 4
                            tick(1)
                            DVE(lambda e, h0=h0, bk=bk: e.tensor_tensor(
                                out=tmpS[:], in0=banks[bk][:].rearrange("p (h t) -> p h t", h=4),
                                in1=vec_t[:, LNG, h0:h0 + 4].unsqueeze(2).to_broadcast([P, 4, P]), op=ALU.mult),
                                r=[("ps", bk), "vec"], w=["tmpS"])
                            DVE(lambda e, h0=h0: e.tensor_tensor(out=tmpS[:], in0=tmpS[:], in1=Cm[:, h0:h0 + 4, :], op=ALU.add),
                                r=["tmpS", "Cm"], w=["tmpS"])
                            DVE(lambda e, h0=h0, j=j: e.tensor_tensor(
                                out=gatedT[:, h0:h0 + 4, j * P:(j + 1) * P], in0=tmpS[:],
                                in1=uT[:, h0:h0 + 4, j * P:(j + 1) * P], op=ALU.mult),
                                r=["tmpS", "uT"], w=["gatedT"])
                    for dcp in range(4):
                        if dcp % 2 == 0:
                            slot = load_wb(g * NWB_G + 14 + dcp // 2)
                        tick(2)
                        bk = next_bank()
                        for d2 in range(2):
                            dc = dcp * 2 + d2
                            for hc in range(8):
                                PE(lambda e, dc=dc, d2=d2, hc=hc, bk=bk, slot=slot: e.matmul(
                                    out=banks[bk][:, d2 * GT:(d2 + 1) * GT], lhsT=wb[slot][:, hc, (dc % 4) * P:(dc % 4 + 1) * P],
                                    rhs=gatedT[:, hc, :], start=(hc == 0), stop=(hc == 7)),
                                   r=[("wb", slot), "gatedT"], w=[("ps", bk)])
                        dc0 = dcp * 2
                        tick(1)
                        DVE(lambda e, dc0=dc0, bk=bk: e.tensor_tensor(
                            out=tmpM[:], in0=banks[bk][:].rearrange("p (a t) -> p a t", a=2),
                            in1=gT[:, 8 + dc0:8 + dc0 + 2, :], op=ALU.mult),
                            r=[("ps", bk), "gT"], w=["tmpM"])
                        DVE(lambda e, dc0=dc0: e.tensor_tensor(out=M32[:, dc0:dc0 + 2, :], in0=M32[:, dc0:dc0 + 2, :],
                                                               in1=tmpM[:], op=ALU.add),
                            r=["tmpM", "M32"], w=["M32"])

                    for j in range(NJ):
                        bks = [next_bank(), next_bank()]
                        for a in range(4):
                            bk = bks[a // 2]
                            for kc in range(2):
                                PE(lambda e, a=a, kc=kc, j=j, bk=bk: e.matmul(
                                    out=banks[bk][:, (a % 2) * NMEM:(a % 2 + 1) * NMEM],
                                    lhsT=qT[:, 2 * a + kc, j * P:(j + 1) * P], rhs=kT[:, 2 * a + kc, :],
                                    start=(kc == 0), stop=(kc == 1)),
                                   r=["qT", "kT"], w=[("ps", bk)])
                        for hb_ in range(2):
                            bk = bks[hb_]
                            tick(1)
                            DVE(lambda e, j=j, hb_=hb_, bk=bk: e.tensor_reduce(
                                out=smx[j][:, hb_ * 2:hb_ * 2 + 2], in_=banks[bk][:].rearrange("p (a m) -> p a m", a=2),
                                axis=AX.X, op=ALU.max),
                                r=[("ps", bk)], w=[("smx", j, hb_)])
                            DVE(lambda e, j=j, hb_=hb_: e.tensor_scalar(
                                out=smx[j][:, 4 + hb_ * 2:4 + hb_ * 2 + 2], in0=smx[j][:, hb_ * 2:hb_ * 2 + 2],
                                scalar1=-1.0 / 16.0, scalar2=None, op0=ALU.mult),
                                r=[("smx", j, hb_)], w=[("smxn", j, hb_)])
                        for a in range(4):
                            bk = bks[a // 2]
                            ACT(lambda e, a=a, j=j, bk=bk: e.activation(
                                out=ex[j][:, a, :], in_=banks[bk][:, (a % 2) * NMEM:(a % 2 + 1) * NMEM], func=AF.Exp,
                                bias=smx[j][:, 4 + a:5 + a], scale=1.0 / 16.0, accum_out=smx[j][:, 8 + a:9 + a]),
                                r=[("ps", bk), ("smxn", j, a // 2)], w=[("ex", j), ("sse", j, a)])
                        tick(1)
                        DVE(lambda e, j=j: e.reciprocal(out=smx[j][:, 12:16], in_=smx[j][:, 8:12]),
                            r=[("sse", j, a) for a in range(4)], w=[("srs", j)])
                        DVE(lambda e, j=j: e.tensor_tensor(
                            out=ex[j][:], in0=ex[j][:], in1=smx[j][:, 12:16].unsqueeze(2).to_broadcast([P, 4, NMEM]),
                            op=ALU.mult),
                            r=[("ex", j), ("srs", j)], w=[("ex", j)])
                        bk = next_bank()
                        psb = banks[bk][:].bitcast(BF16)
                        for a in range(4):
                            for mc in range(2):
                                o = (a * 2 + mc) * P
                                PE(lambda e, a=a, mc=mc, o=o, j=j, psb=psb: e.transpose(
                                    out=psb[:, o:o + P], in_=ex[j][:, a, mc * P:(mc + 1) * P], identity=ident[:]),
                                   r=[("ex", j), "ident"], w=[("ps", bk)])
                        ACT(lambda e, j=j, psb=psb: e.activation(
                            out=probsT[:, :, j * P:(j + 1) * P], in_=psb[:].rearrange("p (c t) -> p c t", c=8), func=AF.Copy),
                            r=[("ps", bk)], w=["probsT"])
                    for hcp in range(4):
                        bk = next_bank()
                        for h2 in range(2):
                            hc = hcp * 2 + h2
                            a = hc // 2
                            for mc in range(2):
                                PE(lambda e, hc=hc, h2=h2, a=a, mc=mc, bk=bk: e.matmul(
                                    out=banks[bk][:, h2 * GT:(h2 + 1) * GT], lhsT=vmem[:, mc, hc * P:(hc + 1) * P],
                                    rhs=probsT[:, a * 2 + mc, :], start=(mc == 0), stop=(mc == 1)),
                                   r=["vmem", "probsT"], w=[("ps", bk)])
                        ACT(lambda e, hcp=hcp, bk=bk: e.activation(
                            out=oT[:, hcp * 2:hcp * 2 + 2, :], in_=banks[bk][:].rearrange("p (a t) -> p a t", a=2), func=AF.Copy),
                            r=[("ps", bk)], w=["oT"])
                    for dcp in range(4):
                        if dcp % 2 == 0:
                            slot = load_wb(g * NWB_G + 16 + dcp // 2)
                        tick(2)
                        bk = next_bank()
                        for d2 in range(2):
                            dc = dcp * 2 + d2
                            for hc in range(8):
                                PE(lambda e, dc=dc, d2=d2, hc=hc, bk=bk, slot=slot: e.matmul(
                                    out=banks[bk][:, d2 * GT:(d2 + 1) * GT], lhsT=wb[slot][:, hc, (dc % 4) * P:(dc % 4 + 1) * P],
                                    rhs=oT[:, hc, :], start=(hc == 0), stop=(hc == 7)),
                                   r=[("wb", slot), "oT"], w=[("ps", bk)])
                        dc0 = dcp * 2
                        tick(1)
                        DVE(lambda e, dc0=dc0, bk=bk: e.tensor_tensor(
                            out=tmpM[:], in0=banks[bk][:].rearrange("p (a t) -> p a t", a=2),
                            in1=gT[:, 16 + dc0:16 + dc0 + 2, :], op=ALU.mult),
                            r=[("ps", bk), "gT"], w=["tmpM"])
                        DVE(lambda e, dc0=dc0: e.tensor_tensor(out=Mb[:, dc0:dc0 + 2, :], in0=M32[:, dc0:dc0 + 2, :],
                                                               in1=tmpM[:], op=ALU.add),
                            r=["tmpM", "M32"], w=["Mb"])

                    xsl = []
                    for j in range(NJ):
                        T = t0 + j
                        sl = xslot[0] % 2
                        xslot[0] += 1
                        xsl.append(sl)
                        DMA(lambda e, T=T, sl=sl: e.dma_start(out=xbuf[sl][:], in_=x[T * P:(T + 1) * P, :]), w=[("xbuf", sl)])
                    for n in range(2):
                        slot = load_wb(g * NWB_G + 18 + n)
                        tick(2)
                        for j in range(NJ):
                            sl = xsl[j]
                            bk = next_bank()
                            for c in range(8):
                                PE(lambda e, c=c, j=j, bk=bk, slot=slot: e.matmul(
                                    out=banks[bk][:], lhsT=Mb[:, c, j * P:(j + 1) * P], rhs=wb[slot][:, c, :],
                                    start=(c == 0), stop=(c == 7)),
                                   r=["Mb", ("wb", slot)], w=[("ps", bk)])
                            tick(1)
                            DVE(lambda e, j=j, n=n, bk=bk, sl=sl: e.tensor_tensor(
                                out=gv[j][:, n * 512:(n + 1) * 512], in0=banks[bk][:], in1=xbuf[sl][:, n * 512:(n + 1) * 512],
                                op=ALU.add),
                                r=[("ps", bk), ("xbuf", sl)], w=[("gv", j)])
                    for j in range(NJ):
                        T = t0 + j
                        DMA(lambda e, T=T, j=j: e.dma_start(out=h_scr[T * P:(T + 1) * P, :], in_=gv[j][:]),
                            r=[("gv", j)], w=["h_scr"])
                    sp_ = g % 2
                    while len(bgq) > 1:
                        for _ in bgq[0]:
                            pass
                        bgq.pop(0)
                    bA, bB = next_bank(), next_bank()
                    for j in range(NJ):
                        rms_tile(gv[j], ("gv", j), xn[j], ("xn", j), ms[j], "ms%d" % j)
                        transpose_tile(xn[j], ("xn", j), bA, bB, j, GT)
                    evac_T(n2T, "n2T", bA, bB, G2, GT)
                    DMA(lambda e, g=g: e.dma_start(out=n2t_v[g], in_=n2T[:].rearrange("p c t -> p (c t)")),
                        r=["n2T"], w=["n2t_scr"])
                    for n in range(4):
                        slot = load_wb(g * NWB_G + 20 + n)
                        for j in range(NJ):
                            bk = next_bank()
                            for c in range(8):
                                PE(lambda e, c=c, j=j, bk=bk, slot=slot: e.matmul(
                                    out=banks[bk][:], lhsT=n2T[:, c, j * P:(j + 1) * P], rhs=wb[slot][:, c, :],
                                    start=(c == 0), stop=(c == 7)),
                                   r=["n2T", ("wb", slot)], w=[("ps", bk)])
                            ACT(lambda e, j=j, n=n, bk=bk, sp_=sp_: e.activation(out=Sb[sp_ * 2 + j][:, n * 512:(n + 1) * 512],
                                                                                 in_=banks[bk][:], func=AF.Copy),
                                r=[("ps", bk)], w=[("Sb", sp_ * 2 + j, n)])
                    bgq.append(routing_gen(t0, sp_))
                tick(None)
            sch.barrier()
        else:
            cast_experts()

        if "B2" in phases:
            esC = ExitStack()
            with esC:
                sbC = lambda name, shape, dt: esC.enter_context(nc.sbuf_tensor(name, list(shape), dt))
                iotaE = sbC("iotaE", [P, P], BF16)
                DMA(lambda e: e.dma_start(out=iotaE[:], in_=c_iota[:, :]), w=["iotaE"], q="pool")
                hbC = sbC("hbC", [P, D], F32)
                n2TC = [sbC("n2TC%d" % i, [P, 8, GT], BF16) for i in range(2)]
                JTC = [sbC("JTC%d" % i, [P, 3, P], F32) for i in range(4)]
                TP = 8
                NOH = 2
                OH1s = [sbC("OH1s%d" % i, [P, TP, P], BF16) for i in range(NOH)]
                OH2s = [sbC("OH2s%d" % i, [P, TP, P], BF16) for i in range(NOH)]
                Gf = [sbC("Gf%d" % i, [P, GT, P], BF16) for i in range(2)]
                NUB, NVB = 3, 4
                ub = [sbC("ub%d" % i, [P, 2, 8, P], BF16) for i in range(NUB)]
                vb = [sbC("vb%d" % i, [P, 2, D], BF16) for i in range(NVB)]
                NHA = 3
                Hg = [sbC("Hg%d" % i, [P, 2, GT], BF16) for i in range(NHA)]
                At = [sbC("At%d" % i, [P, 2, GT], BF16) for i in range(NHA)]
                ms3 = sbC("ms3", [P, 4], F32)
                ob = sbC("ob", [P, D], F32)

                n2t_v = n2t_scr.rearrange("(g p) f -> g p f", p=P)
                ut_v = ut_bf.rearrange("(b p) (c e) -> p b c e", p=P, c=8)
                v_v = v_bf.rearrange("(b e) d -> e b d", e=P)
                YB = [[0, 1], [2, 3]]
                HB = [4, 5]
                MB = [6, 7]
                NP2 = P // 2
                uq = [0]
                vq = [0]
                ohc = [0]

                def load_group_inputs(gg):
                    par = gg % 2
                    DMA(lambda e, gg=gg, par=par: e.dma_start(out=n2TC[par][:].rearrange("p c t -> p (c t)"), in_=n2t_v[gg]),
                        r=["n2t_scr"], w=[("n2TC", par)])
                    for j in range(NJ):
                        T = gg * NJ + j
                        DMA(lambda e, T=T, j=j, par=par: e.dma_start(out=JTC[par * 2 + j][:].rearrange("p w t -> p (w t)"),
                                                                     in_=jt_scr[T * P:(T + 1) * P, :]),
                            r=["jt_scr"], w=[("JTC", par * 2 + j)])

                def prefetch_u(hi):
                    while uq[0] <= min(hi, NG * NP2 - 1):
                        idx = uq[0]
                        uq[0] += 1
                        b0 = (idx % NP2) * 2
                        DMA(lambda e, b0=b0, us=idx % NUB: e.dma_start(out=ub[us][:], in_=ut_v[:, b0:b0 + 2, :, :]),
                            r=["ut_bf"], w=[("ub", idx % NUB)])

                def prefetch_v(hi):
                    while vq[0] <= min(hi, NG * NP2 - 1):
                        idx = vq[0]
                        vq[0] += 1
                        b0 = (idx % NP2) * 2
                        DMA(lambda e, b0=b0, vs=idx % NVB: e.dma_start(out=vb[vs][:], in_=v_v[:, b0:b0 + 2, :]),
                            r=["v_bf"], w=[("vb", idx % NVB)])

                def build_steps(gg):
                    par = gg % 2
                    Gd = Gf[par]
                    deferred = [None]
                    for j in range(NJ):
                        jt = JTC[par * 2 + j]
                        jk = ("JTC", par * 2 + j)
                        for pc in range(P // TP):
                            sl = ohc[0] % NOH
                            ohc[0] += 1
                            tq = pc * TP
                            iob = iotaE[:].unsqueeze(1).to_broadcast([P, TP, P])
                            DVE(lambda e, sl=sl, tq=tq, iob=iob, jt=jt: e.tensor_tensor(
                                out=OH1s[sl][:], in0=iob, in1=jt[:, 0, tq:tq + TP].unsqueeze(2).to_broadcast([P, TP, P]),
                                op=ALU.is_equal),
                                r=["iotaE", jk], w=[("OH1", sl)])
                            DVE(lambda e, sl=sl, tq=tq, iob=iob, jt=jt: e.tensor_tensor(
                                out=OH2s[sl][:], in0=iob, in1=jt[:, 1, tq:tq + TP].unsqueeze(2).to_broadcast([P, TP, P]),
                                op=ALU.is_equal),
                                r=["iotaE", jk], w=[("OH2", sl)])
                            POOL(lambda e, sl=sl, tq=tq, jt=jt: e.tensor_tensor(
                                out=OH2s[sl][:], in0=OH2s[sl][:], in1=jt[:, 2, tq:tq + TP].unsqueeze(2).to_broadcast([P, TP, P]),
                                op=ALU.mult),
                                r=[("OH2", sl), jk], w=[("OH2", sl)])
                            def pe_part(sl=sl, j=j, tq=tq, Gd=Gd, par=par):
                                for q4 in range(TP // 4):
                                    bk = next_bank(MB)
                                    for tt in range(4):
                                        tl = q4 * 4 + tt
                                        PE(lambda e, sl=sl, tl=tl, tt=tt, bk=bk: e.matmul(
                                            out=banks[bk][:, tt * P:(tt + 1) * P], lhsT=OH2s[sl][:, tl, :], rhs=OH1s[sl][:, tl, :],
                                            start=True, stop=True),
                                           r=[("OH1", sl), ("OH2", sl)], w=[("ps", bk)])
                                    tok = j * P + tq + q4 * 4
                                    ACT(lambda e, tok=tok, bk=bk, Gd=Gd: e.activation(
                                        out=Gd[:, tok:tok + 4, :], in_=banks[bk][:].rearrange("p (t e) -> p t e", t=4), func=AF.Copy),
                                        r=[("ps", bk)], w=[("Gf", par)])
                            if deferred[0] is not None:
                                deferred[0]()
                            deferred[0] = pe_part
                            yield
                    if deferred[0] is not None:
                        deferred[0]()
                        deferred[0] = None
                load_group_inputs(0)
                prefetch_u(1)
                prefetch_v(1)
                for _ in build_steps(0):
                    pass
                LAG = 2
                for g in range(NG):
                    t0 = g * NJ
                    par = g % 2
                    bg = None
                    if g + 1 < NG:
                        load_group_inputs(g + 1)
                        bg = build_steps(g + 1)
                    nsteps = NJ * (P // TP)
                    done_steps = 0
                    pend = []
                    for bp in range(NP2 + LAG):
                        gq = g * NP2 + bp
                        if bp < NP2:
                            b0 = bp * 2
                            prefetch_u(gq)
                            us = gq % NUB
                            ha = bp % NHA
                            hbk = HB[bp % 2]
                            for k in range(2):
                                for c in range(8):
                                    PE(lambda e, us=us, c=c, k=k, hbk=hbk, par=par: e.matmul(
                                        out=banks[hbk][:, k * GT:(k + 1) * GT], lhsT=ub[us][:, k, c, :], rhs=n2TC[par][:, c, :],
                                        start=(c == 0), stop=(c == 7)),
                                       r=[("ub", us), ("n2TC", par)], w=[("ps", hbk)])
                            ACT(lambda e, ha=ha, hbk=hbk: e.activation(
                                out=Hg[ha][:], in_=banks[hbk][:].rearrange("p (k t) -> p k t", k=2), func=AF.Gelu),
                                r=[("ps", hbk)], w=[("Hg", ha)])
                            gsrc = Gf[par][:, :, b0:b0 + 2].rearrange("p t k -> p k t")
                            DVE(lambda e, ha=ha, gsrc=gsrc: e.tensor_tensor(out=At[ha][:], in0=Hg[ha][:], in1=gsrc, op=ALU.mult),
                                r=[("Hg", ha), ("Gf", par)], w=[("At", ha)])
                            pend.append((ha, gq, b0))
                        if bp >= LAG:
                            pha, pgq, pb0 = pend.pop(0)
                            prefetch_v(pgq)
                            pvs = pgq % NVB
                            for k in range(2):
                                b = pb0 + k
                                for j in range(NJ):
                                    for n in range(2):
                                        ybk = YB[j][n]
                                        PE(lambda e, pha=pha, pvs=pvs, k=k, j=j, n=n, ybk=ybk, b=b: e.matmul(
                                            out=banks[ybk][:], lhsT=At[pha][:, k, j * P:(j + 1) * P],
                                            rhs=vb[pvs][:, k, n * 512:(n + 1) * 512], start=(b == 0), stop=(b == P - 1)),
                                           r=[("At", pha), ("vb", pvs)], w=[("ps", ybk)])
                            prefetch_v(pgq + NVB - 1)
                        if bp < NP2:
                            prefetch_u(gq + NUB - 1)
                        if bg is not None and bp < NP2:
                            want = ((bp + 1) * nsteps) // NP2
                            while done_steps < want:
                                next(bg, None)
                                done_steps += 1
                    if bg is not None:
                        for _ in bg:
                            pass
                    for j in range(NJ):
                        T = t0 + j
                        DMA(lambda e, T=T: e.dma_start(out=hbC[:], in_=h_scr[T * P:(T + 1) * P, :]),
                            r=["h_scr"], w=["hbC"])
                        for n in range(2):
                            ybk = YB[j][n]
                            DVE(lambda e, n=n, ybk=ybk: e.tensor_tensor(
                                out=hbC[:, n * 512:(n + 1) * 512], in0=banks[ybk][:], in1=hbC[:, n * 512:(n + 1) * 512],
                                op=ALU.add),
                                r=[("ps", ybk), "hbC"], w=["hbC"])
                        ACT(lambda e: e.activation(out=ob[:], in_=hbC[:], func=AF.Square, scale=1.0 / 32.0,
                                                   accum_out=ms3[:, 0:1]),
                            r=["hbC"], w=["ob", "ms3"])
                        ACT(lambda e: e.activation(out=ms3[:, 2:3], in_=ms3[:, 0:1], func=AF.Sqrt, bias=epsT[:, 0:1], scale=1.0),
                            r=["ms3", "epsT"], w=["ms3s"])
                        DVE(lambda e: e.reciprocal(out=ms3[:, 1:2], in_=ms3[:, 2:3]), r=["ms3s"], w=["ms3r"])
                        DVE(lambda e: e.scalar_tensor_tensor(out=ob[:], in0=hbC[:], scalar=ms3[:, 1:2], in1=fgB[:],
                                                             op0=ALU.mult, op1=ALU.mult),
                            r=["hbC", "ms3r", "fgB"], w=["ob"])
                        out_dmas.append(DMA(lambda e, T=T: e.dma_start(out=out[T * P:(T + 1) * P, :], in_=ob[:]),
                                            r=["ob"], w=["out"]))
            sch.barrier()

        sch.barrier()
        sch.final_wait(out_dmas)
        n_wait = sch.lower()
    return nc


def _vecT(v):
    return np.ascontiguousarray(np.asarray(v, np.float32).reshape(8, P).T)


def make_in_maps(inputs, n_cores=8, n_tok=S):
    f = lambda k: np.asarray(inputs[k], np.float32)
    consts = _const_tables()
    vecs = np.stack([_vecT(f("norm1_gain")[0]), _vecT(f("norm2_gain")[0]), _vecT(f("mem_norm_gain")[0]),
                     _vecT(f("pool_scale")[0]), _vecT(f("sgu_ln_gain")[0]), _vecT(f("sgu_ln_bias")[0])], axis=1)
    U = f("peer_u")[0]
    UTh = np.ascontiguousarray(U.reshape(P, P, 8, P).transpose(0, 3, 2, 1)).reshape(P * P, 8 * P)
    wq = f("peer_w_q")[0]
    wqT = np.ascontiguousarray(wq.reshape(D, 16, P).transpose(2, 1, 0))
    keysT = np.ascontiguousarray(np.stack([f("peer_keys1")[0].T, f("peer_keys2")[0].T], axis=1))
    shared = {
        "w_in": np.ascontiguousarray(f("w_in")[0]),
        "vecs": np.ascontiguousarray(vecs),
        "fg": np.ascontiguousarray(f("final_norm_gain").reshape(1, D)),
        "pool_w": np.ascontiguousarray(f("pool_w")[0]),
        "wsT": np.ascontiguousarray(f("sgu_w_s")[0].transpose(2, 0, 1)),
        "bs": np.ascontiguousarray(f("sgu_b_s")[0].reshape(1, 8 * P)),
        "swo": np.ascontiguousarray(f("sgu_w_out")[0]),
        "wkv": np.ascontiguousarray(f("xa_w_kv")[0]),
        "xwo": np.ascontiguousarray(f("xa_w_out")[0]),
        "wout": np.ascontiguousarray(f("w_out")[0]),
        "wqT": wqT,
        "keysT": keysT,
        "UTh": UTh,
        "Vh": np.ascontiguousarray(f("peer_v")[0]),
    }
    shared.update(consts)
    x = f("x")
    mem = f("mem")
    maps = []
    for b in range(n_cores):
        m = dict(shared)
        m["x"] = np.ascontiguousarray(x[b, :n_tok])
        m["mem"] = np.ascontiguousarray(mem[b])
        maps.append(m)
    return maps


def kernel(**inputs):
    nc = build()
    in_maps = make_in_maps(inputs)
    res = run_bass_kernel_spmd(nc, in_maps, core_ids=list(range(8)))
    return np.stack([np.asarray(r["out"], np.float32) for r in res.results], axis=0)
```

```python
import numpy as np
from contextlib import ExitStack
import concourse.bass as bass
import concourse.mybir as mybir
from concourse.bass_utils import run_bass_kernel_spmd

F32 = mybir.dt.float32
BF16 = mybir.dt.bfloat16
U32 = mybir.dt.uint32
AF = mybir.ActivationFunctionType
ALU = mybir.AluOpType
AX = mybir.AxisListType

P = 128
D = 1024
S = 4096
NMEM = 256
INW = 7168
NEXP = 16384
GT = 256
NJ = GT // P
RMS_EPS = 1e-6
LN_EPS = 1e-5
NEG = -1.0e30


class Sched:
    STREAMS = ("pe", "act", "dve", "pool", "sp")

    def __init__(self, nc, es, n_dma_sems=24):
        self.nc = nc
        self.es = es
        self.recs = []
        self.W = {}
        self.R = {}
        self.last = {}
        self.dmas_since_barrier = []
        self.n_dma_sems = n_dma_sems

    def op(self, stream, fn, reads=(), writes=(), dma=False):
        i = len(self.recs)
        deps = {}
        for k in reads:
            for w in self.W.get(k, ()):
                deps[w] = "raw"
        for k in writes:
            rs = self.R.get(k)
            if rs:
                for r in rs:
                    deps.setdefault(r, "war")
                for w in self.W.get(k, ()):
                    deps.setdefault(w, "waw")
                self.W[k] = []
                self.R[k] = []
            else:
                for w in self.W.get(k, ()):
                    deps.setdefault(w, "waw")
        for k in reads:
            self._push(self.R.setdefault(k, []), i, stream, dma)
        for k in writes:
            self._push(self.W.setdefault(k, []), i, stream, dma)
        self.recs.append(dict(stream=stream, fn=fn, dma=dma, deps=deps, need=False, ev=None))
        if fn is not None:
            self.last[stream] = i
        if dma:
            self.dmas_since_barrier.append(i)
        return i

    def _push(self, lst, i, stream, dma):
        if lst and not dma:
            j = lst[-1]
            rj = self.recs[j]
            if rj["stream"] == stream and not rj["dma"]:
                lst[-1] = i
                return
        lst.append(i)

    def barrier(self):
        lasts = dict(self.last)
        dmas = list(self.dmas_since_barrier)
        for s in self.STREAMS:
            deps = {}
            for s2, i in lasts.items():
                if s2 != s or self.recs[i]["dma"]:
                    deps[i] = "raw"
            for i in dmas:
                deps[i] = "raw"
            self.recs.append(dict(stream=s, fn=None, dma=False, deps=deps, need=False, ev=None))
        self.W = {}
        self.R = {}
        self.dmas_since_barrier = []

    def final_wait(self, rec_ids):
        deps = {i: "raw" for i in rec_ids}
        self.recs.append(dict(stream="sp", fn=None, dma=False, deps=deps, need=False, ev=None))

    def lower(self):
        nc = self.nc
        recs = self.recs
        eng = {"pe": nc.tensor, "act": nc.scalar, "dve": nc.vector, "pool": nc.gpsimd, "sp": nc.sync}
        for i, r in enumerate(recs):
            waits = []
            for d, kind in r["deps"].items():
                rd = recs[d]
                if rd["fn"] is None:
                    continue
                if rd["stream"] == r["stream"] and not rd["dma"] and not r["dma"]:
                    if kind != "raw" or r["stream"] == "pe":
                        continue
                waits.append(d)
                rd["need"] = True
            r["waits"] = sorted(waits)
        cnt = {s: 0 for s in self.STREAMS}
        sems = {s: self.es.enter_context(nc.semaphore("sem_" + s)) for s in self.STREAMS}
        semgen = {s: 0 for s in self.STREAMS}
        dsems = {q: [self.es.enter_context(nc.semaphore("dsem_%s%d" % (q, k))) for k in range(n)]
                 for q, n in (("sp", 16), ("pool", 8))}
        dvals = {q: [0] * len(v) for q, v in dsems.items()}
        dnexts = {q: 0 for q in dsems}
        seen = {s: {} for s in self.STREAMS}
        n_wait = 0
        for r in recs:
            s = r["stream"]
            e = eng[s]
            sn = seen[s]
            for d in r["waits"]:
                sem, val = recs[d]["ev"]
                if sn.get(id(sem), 0) >= val:
                    continue
                e.wait_ge(sem, val)
                n_wait += 1
                sn[id(sem)] = val
            if r["fn"] is None:
                continue
            if r["dma"]:
                dsem, dval = dsems[s], dvals[s]
                k = dnexts[s]
                dnexts[s] = (k + 1) % len(dsem)
                if dval[k] > 0 and sn.get(id(dsem[k]), 0) < dval[k]:
                    e.wait_ge(dsem[k], dval[k])
                    sn[id(dsem[k])] = dval[k]
                ins = r["fn"](e)
                dval[k] += 16
                ins.then_inc(dsem[k], 16)
                r["ev"] = (dsem[k], dval[k])
            else:
                ins = r["fn"](e)
                if r["need"]:
                    if cnt[s] >= 30000:
                        semgen[s] += 1
                        sems[s] = self.es.enter_context(nc.semaphore("sem_%s_%d" % (s, semgen[s])))
                        cnt[s] = 0
                    cnt[s] += 1
                    ins.then_inc(sems[s], 1)
                    r["ev"] = (sems[s], cnt[s])
        return n_wait


def _const_tables():
    windows = (2, 4, 8, 16)
    s = np.arange(P)[:, None]
    t = np.arange(P)[None, :]
    bands = np.zeros((12, P, P), np.float32)
    for g, w in enumerate(windows):
        cur = ((s <= t) & (s > t - w)).astype(np.float32) / w
        bands[g] = cur - np.eye(P, dtype=np.float32)
        bands[4 + g] = (s > P + t - w).astype(np.float32) / w
        cnt = np.minimum(t + 1, w).astype(np.float32)
        bands[8 + g] = ((s <= t) & (s > t - w)).astype(np.float32) / cnt - np.eye(P, dtype=np.float32)
    consts = {
        "c_ident": np.eye(P, dtype=np.float32),
        "c_bands": np.ascontiguousarray(bands.transpose(1, 0, 2)),
        "c_trilT": (s <= t).astype(np.float32),
        "c_iota": np.tile(np.arange(P, dtype=np.float32)[None, :], (P, 1)),
        "c_iota16": np.tile(np.arange(16, dtype=np.float32)[None, :], (P, 8 * 16)),
    }
    return consts


def build(n_tok=S, debug=False, phases=("A", "B2")):
    nc = bass.Bass("TRN2", target_bir_lowering=False)
    NG = n_tok // GT
    NT = n_tok // P

    def din(name, shape, dt=F32):
        return nc.dram_tensor(name, list(shape), dt, kind="ExternalInput").ap()

    x = din("x", [n_tok, D])
    mem = din("mem", [NMEM, D])
    w_in = din("w_in", [D, INW])
    vecs = din("vecs", [P, 6, 8])
    fg = din("fg", [1, D])
    pool_w = din("pool_w", [4, 256, 256])
    wsT = din("wsT", [P, 8, P])
    bs = din("bs", [1, 8 * P])
    swo = din("swo", [D, D])
    wkv = din("wkv", [D, 2 * D])
    xwo = din("xwo", [D, D])
    wout = din("wout", [D, D])
    wqT = din("wqT", [P, 16, D])
    keysT = din("keysT", [P, 2, P])
    UTh = din("UTh", [P * P, 8 * P])
    Vh = din("Vh", [NEXP, D])
    c_ident = din("c_ident", [P, P])
    c_bands = din("c_bands", [P, 12, P])
    c_trilT = din("c_trilT", [P, P])
    c_iota = din("c_iota", [P, P])
    c_iota16 = din("c_iota16", [P, 8 * 16 * 16])

    out = nc.dram_tensor("out", [n_tok, D], F32, kind="ExternalOutput").ap()
    kind_dbg = "ExternalOutput" if debug else "Internal"
    h_scr = nc.dram_tensor("h_scr", [n_tok, D], F32, kind=kind_dbg).ap()
    jt_scr = nc.dram_tensor("jt_scr", [NT * P, 3 * P], F32, kind=kind_dbg).ap()
    n2t_scr = nc.dram_tensor("n2t_scr", [NG * P, 8 * GT], BF16, kind="Internal").ap()
    NWCOL = INW + 3 * D + 2048
    win_bf = nc.dram_tensor("win_bf", [D, NWCOL], BF16, kind="Internal").ap()
    ut_bf = nc.dram_tensor("ut_bf", [P * P, 8 * P], BF16, kind="Internal").ap()
    v_bf = nc.dram_tensor("v_bf", [NEXP, D], BF16, kind="Internal").ap()

    es_all = ExitStack()
    sch = Sched(nc, es_all)

    def PE(fn, r=(), w=()):
        return sch.op("pe", fn, r, w)

    def ACT(fn, r=(), w=()):
        return sch.op("act", fn, r, w)

    def DVE(fn, r=(), w=()):
        return sch.op("dve", fn, r, w)

    def POOL(fn, r=(), w=()):
        return sch.op("pool", fn, r, w)

    def DMA(fn, r=(), w=(), q="sp"):
        return sch.op(q, fn, r, w, dma=True)

    out_dmas = []

    with es_all:
        es = es_all
        sb = lambda name, shape, dt: es.enter_context(nc.sbuf_tensor(name, list(shape), dt))
        banks = [es.enter_context(nc.psum_tensor("bank%d" % i, [P, 512], F32)) for i in range(8)]
        bank_rr = [0]

        def next_bank(pool=range(8)):
            pool = list(pool)
            b = pool[bank_rr[0] % len(pool)]
            bank_rr[0] += 1
            return b

        ident = sb("ident", [P, P], BF16)
        vec_t = sb("vec_t", [P, 6, 8], F32)
        fgB = sb("fgB", [P, D], F32)
        epsT = sb("epsT", [P, 2], F32)
        POOL(lambda e: e.memset(epsT[:, 0:1], RMS_EPS), w=["epsT"])
        POOL(lambda e: e.memset(epsT[:, 1:2], LN_EPS), w=["epsT"])
        DMA(lambda e: e.dma_start(out=ident[:], in_=c_ident[:, :]), w=["ident"], q="pool")
        ident_f = sb("ident_f", [P, P], F32)
        DMA(lambda e: e.dma_start(out=ident_f[:], in_=c_ident[:, :]), w=["ident_f"])
        DMA(lambda e: e.dma_start(out=vec_t[:], in_=vecs[:, :, :]), w=["vec"])
        DMA(lambda e: e.dma_start(out=fgB[:], in_=fg.partition_broadcast(P)), w=["fgB"])
        G1, G2, GM, PSC, LNG, LNB = range(6)

        for c in range(8):
            DMA(lambda e, c=c: e.dma_start(out=win_bf[c * P:(c + 1) * P, 0:INW], in_=w_in[c * P:(c + 1) * P, :]),
                w=["win_bf"], q="pool")
        for wi, wsrc in enumerate((swo, xwo, wout)):
            for c in range(0, 8, 2):
                DMA(lambda e, c=c, wi=wi, wsrc=wsrc: e.dma_start(
                    out=win_bf[c * P:(c + 2) * P, INW + wi * D:INW + (wi + 1) * D], in_=wsrc[c * P:(c + 2) * P, :]),
                    w=["win_bf"], q="pool")

        def cast_experts(b_lo=0, b_hi=P, dep=()):
            for b in range(b_lo, b_hi):
                DMA(lambda e, b=b: e.dma_start(out=ut_bf[b * P:(b + 1) * P, :], in_=UTh[b * P:(b + 1) * P, :]),
                    r=list(dep), w=["ut_bf"], q="pool")
                DMA(lambda e, b=b: e.dma_start(out=v_bf[b * P:(b + 1) * P, :], in_=Vh[b * P:(b + 1) * P, :]),
                    r=list(dep), w=["v_bf"], q="pool")

        def rms_tile(src, srckey, xn, xnkey, ms, mskey):
            ACT(lambda e: e.activation(out=xn[:], in_=src[:], func=AF.Square, scale=1.0 / 32.0,
                                       accum_out=ms[:, 0:1]),
                r=[srckey], w=[xnkey, mskey])
            ACT(lambda e: e.activation(out=ms[:, 1:2], in_=ms[:, 0:1], func=AF.Sqrt, bias=epsT[:, 0:1], scale=1.0),
                r=[mskey, "epsT"], w=[mskey + "s"])
            DVE(lambda e: e.reciprocal(out=ms[:, 2:3], in_=ms[:, 1:2]), r=[mskey + "s"], w=[mskey + "r"])
            ACT(lambda e: e.activation(out=xn[:], in_=src[:], func=AF.Copy, scale=ms[:, 2:3]),
                r=[srckey, mskey + "r"], w=[xnkey])

        def transpose_tile(xn, xnkey, bA, bB, j, ncol):
            for c in range(8):
                bk = bA if c < 4 else bB
                psb = banks[bk][:].bitcast(BF16)
                o = (c % 4) * ncol + j * P
                PE(lambda e, c=c, psb=psb, o=o: e.transpose(out=psb[:, o:o + P], in_=xn[:, c * P:(c + 1) * P],
                                                           identity=ident[:]),
                   r=[xnkey, "ident"], w=[("ps", bk)])

        def evac_T(dst, dstkey, bA, bB, gidx, ncol):
            for k, bk in enumerate((bA, bB)):
                psb = banks[bk][:].bitcast(BF16)
                DVE(lambda e, k=k, psb=psb: e.tensor_tensor(
                    out=dst[:, 4 * k:4 * k + 4, :],
                    in0=psb[:, 0:4 * ncol].rearrange("p (c t) -> p c t", c=4),
                    in1=vec_t[:, gidx, 4 * k:4 * k + 4].unsqueeze(2).to_broadcast([P, 4, ncol]),
                    op=ALU.mult),
                    r=[("ps", bk), "vec"], w=[dstkey])

        if "A" in phases:
            esA = ExitStack()
            with esA:
                sbA = lambda name, shape, dt: esA.enter_context(nc.sbuf_tensor(name, list(shape), dt))
                bands = sbA("bands", [P, 12, P], BF16)
                WmT = sbA("WmT", [P, 8, P], BF16)
                Cm = sbA("Cm", [P, 8, P], F32)
                pw_t = sbA("pw_t", [P, 8, 256], BF16)
                kT = sbA("kT", [P, 8, NMEM], BF16)
                vmem = sbA("vmem", [P, 2, D], BF16)
                DMA(lambda e: e.dma_start(out=bands[:], in_=c_bands[:, :, :]), w=["bands"], q="pool")
                DMA(lambda e: e.dma_start(out=pw_t[:], in_=pool_w.rearrange("g (k p) d -> p (g k) d", p=P)), w=["pw"], q="pool")

                esS = ExitStack()
                with esS:
                    sbS = lambda name, shape, dt: esS.enter_context(nc.sbuf_tensor(name, list(shape), dt))
                    wsT_f = sbS("wsT_f", [P, 8, P], F32)
                    tril_f = sbS("tril_f", [P, P], F32)
                    BSb = sbS("BSb", [P, 8, P], F32)
                    ones_bf = sbS("ones_bf", [P, P], BF16)
                    wkv_t = sbS("wkv_t", [P, 8, 2 * D], BF16)
                    memf = [sbS("memf%d" % i, [P, D], F32) for i in range(2)]
                    memn = [sbS("memn%d" % i, [P, D], BF16) for i in range(2)]
                    mms = [sbS("mms%d" % i, [P, 4], F32) for i in range(2)]
                    memnT = sbS("memnT", [P, 8, NMEM], BF16)

                    DMA(lambda e: e.dma_start(out=wsT_f[:], in_=wsT[:, :, :]), w=["wsT_f"])
                    DMA(lambda e: e.dma_start(out=tril_f[:], in_=c_trilT[:, :]), w=["tril_f"])
                    DMA(lambda e: e.dma_start(out=BSb[:].rearrange("p h t -> p (h t)"), in_=bs.partition_broadcast(P)), w=["BSb"])
                    DMA(lambda e: e.dma_start(out=wkv_t[:], in_=wkv.rearrange("(c p) n -> p c n", p=P)), w=["wkv"], q="pool")
                    POOL(lambda e: e.memset(ones_bf[:], 1.0), w=["ones"])
                    DVE(lambda e: e.tensor_tensor(out=WmT[:], in0=wsT_f[:],
                                                  in1=tril_f[:].unsqueeze(1).to_broadcast([P, 8, P]), op=ALU.mult),
                        r=["wsT_f", "tril_f"], w=["WmT"])
                    for half in range(2):
                        bk = next_bank()
                        for hh in range(4):
                            h = half * 4 + hh
                            PE(lambda e, h=h, hh=hh, bk=bk: e.matmul(out=banks[bk][:, hh * P:(hh + 1) * P], lhsT=ones_bf[:],
                                                                     rhs=WmT[:, h, :], start=True, stop=True),
                               r=["ones", "WmT"], w=[("ps", bk)])
                        for hh in range(4):
                            h = half * 4 + hh
                            DVE(lambda e, h=h, hh=hh, bk=bk: e.scalar_tensor_tensor(
                                out=Cm[:, h, :], in0=banks[bk][:, hh * P:(hh + 1) * P], scalar=vec_t[:, LNB, h:h + 1],
                                in1=BSb[:, h, :], op0=ALU.mult, op1=ALU.add),
                                r=[("ps", bk), "vec", "BSb"], w=["Cm"])
                    bA, bB = next_bank(), next_bank()
                    for mt in range(2):
                        DMA(lambda e, mt=mt: e.dma_start(out=memf[mt][:], in_=mem[mt * P:(mt + 1) * P, :]), w=[("memf", mt)])
                        rms_tile(memf[mt], ("memf", mt), memn[mt], ("memn", mt), mms[mt], "mms%d" % mt)
                        transpose_tile(memn[mt], ("memn", mt), bA, bB, mt, NMEM)
                    evac_T(memnT, "memnT", bA, bB, GM, NMEM)
                    for hc in range(8):
                        if hc % 2 == 0:
                            bk = next_bank()
                        for c in range(8):
                            PE(lambda e, hc=hc, c=c, bk=bk: e.matmul(
                                out=banks[bk][:, (hc % 2) * NMEM:(hc % 2 + 1) * NMEM],
                                lhsT=wkv_t[:, c, hc * P:(hc + 1) * P], rhs=memnT[:, c, :], start=(c == 0), stop=(c == 7)),
                               r=["wkv", "memnT"], w=[("ps", bk)])
                        if hc % 2 == 1:
                            ACT(lambda e, hc=hc, bk=bk: e.activation(
                                out=kT[:, hc - 1:hc + 1, :], in_=banks[bk][:].rearrange("p (a m) -> p a m", a=2), func=AF.Copy),
                                r=[("ps", bk)], w=["kT"])
                    for mt in range(2):
                        for n in range(2):
                            bk = next_bank()
                            for c in range(8):
                                PE(lambda e, mt=mt, n=n, c=c, bk=bk: e.matmul(
                                    out=banks[bk][:], lhsT=memnT[:, c, mt * P:(mt + 1) * P],
                                    rhs=wkv_t[:, c, D + n * 512:D + (n + 1) * 512], start=(c == 0), stop=(c == 7)),
                                   r=["wkv", "memnT"], w=[("ps", bk)])
                            ACT(lambda e, mt=mt, n=n, bk=bk: e.activation(out=vmem[:, mt, n * 512:(n + 1) * 512],
                                                                         in_=banks[bk][:], func=AF.Copy),
                                r=[("ps", bk)], w=["vmem"])
                    wqT_t = sbS("wqT_t", [P, 16, D], BF16)
                    keysT_t = sbS("keysT_t", [P, 2, P], BF16)
                    Ws_t = sbS("Ws_t", [P, 8, 2048], BF16)
                    DMA(lambda e: e.dma_start(out=wqT_t[:], in_=wqT[:, :, :]), w=["wqT"], q="pool")
                    DMA(lambda e: e.dma_start(out=keysT_t[:], in_=keysT[:, :, :]), w=["keysT"], q="pool")
                    for dc in range(8):
                        for ib in range(4):
                            bk = next_bank()
                            for ii in range(4):
                                i = ib * 4 + ii
                                PE(lambda e, dc=dc, i=i, ii=ii, bk=bk: e.matmul(
                                    out=banks[bk][:, ii * P:(ii + 1) * P], lhsT=wqT_t[:, i, dc * P:(dc + 1) * P],
                                    rhs=keysT_t[:, i % 2, :], start=True, stop=True),
                                   r=["wqT", "keysT"], w=[("ps", bk)])
                            ACT(lambda e, dc=dc, ib=ib, bk=bk: e.activation(out=Ws_t[:, dc, ib * 512:(ib + 1) * 512],
                                                                          in_=banks[bk][:], func=AF.Copy),
                                r=[("ps", bk)], w=["Ws_t"])
                    DMA(lambda e: e.dma_start(out=win_bf[:, INW + 3 * D:NWCOL].rearrange("(c p) n -> p c n", p=P), in_=Ws_t[:]),
                        r=["Ws_t"], w=["win_bf"])
                sch.barrier()

                xbuf = [sbA("xbuf%d" % i, [P, D], F32) for i in range(2)]
                xn = [sbA("xn%d" % i, [P, D], BF16) for i in range(2)]
                ms = [sbA("ms%d" % i, [P, 4], F32) for i in range(2)]
                nT = sbA("nT", [P, 8, GT], BF16)
                NWB = 3
                wb = [sbA("wb%d" % i, [P, 8, 512], BF16) for i in range(NWB)]
                Pr = [sbA("Pr%d" % i, [P, D], BF16) for i in range(4)]
                gv = [sbA("gv%d" % i, [P, D], F32) for i in range(2)]
                lst = [sbA("lst%d" % i, [P, 8], F32) for i in range(2)]
                zt = [sbA("zt%d" % i, [P, D], BF16) for i in range(2)]
                uT = sbA("uT", [P, 8, GT], BF16)
                qT = sbA("qT", [P, 8, GT], BF16)
                gT = sbA("gT", [P, 24, GT], BF16)
                diffT = sbA("diffT", [P, 8, GT], BF16)
                gatedT = sbA("gatedT", [P, 8, GT], BF16)
                tmpS = sbA("tmpS", [P, 4, P], F32)
                smx = [sbA("smx%d" % i, [P, 16], F32) for i in range(2)]
                ex = [sbA("ex%d" % i, [P, 4, NMEM], BF16) for i in range(2)]
                probsT = sbA("probsT", [P, 8, GT], BF16)
                oT = sbA("oT", [P, 8, GT], BF16)
                M32 = sbA("M32", [P, 8, GT], F32)
                tmpM = sbA("tmpM", [P, 2, GT], F32)
                Mb = sbA("Mb", [P, 8, GT], BF16)
                pace = sbA("pace", [P, 2], F32)
                iota16 = sbA("iota16", [P, 16], F32)
                DMA(lambda e: e.dma_start(out=iota16[:], in_=c_iota[:, 0:16]), w=["iota16"])
                n2T = sbA("n2T", [P, 8, GT], BF16)
                Sb = [sbA("Sb%d" % i, [P, 2048], F32) for i in range(4)]
                S2x = sbA("S2x", [P, 2048], F32)
                Vt = [sbA("Vt%d" % i, [P, 16, 16], F32) for i in range(2)]
                It = [sbA("It%d" % i, [P, 16, 16], U32) for i in range(2)]
                Itf = [sbA("Itf%d" % i, [P, 16, 16], F32) for i in range(2)]
                Bt = [sbA("Bt%d" % i, [P, 8, 16], F32) for i in range(2)]
                CI = [sbA("CI%d" % i, [P, 8, 16], U32) for i in range(2)]
                CIf = [sbA("CIf%d" % i, [P, 3, 128], F32) for i in range(2)]
                CIu = [sbA("CIu%d" % i, [P, 2, 128], U32) for i in range(2)]
                exg = [sbA("exg%d" % i, [P, 128], F32) for i in range(2)]
                zs = [sbA("zs%d" % i, [P, 16], F32) for i in range(2)]
                JB = [sbA("JB%d" % i, [P, 3, 128], F32) for i in range(2)]
                JT = [sbA("JT%d" % i, [P, 3, 128], F32) for i in range(2)]
                n2t_v = n2t_scr.rearrange("(g p) f -> g p f", p=P)
                NWB_G = 24

                win_v = win_bf.rearrange("(c p) n -> p c n", p=P)
                wb_ctr = [0]

                def load_wb(gb):
                    while wb_ctr[0] <= min(gb + NWB - 1, NG * NWB_G - 1):
                        idx = wb_ctr[0]
                        wb_ctr[0] += 1
                        DMA(lambda e, n=idx % NWB_G, slot=idx % NWB: e.dma_start(out=wb[slot][:], in_=win_v[:, :, n * 512:(n + 1) * 512]),
                            r=["win_bf"], w=[("wb", idx % NWB)])
                    return gb % NWB

                bgq = []

                def tick(k):
                    n = 0
                    while bgq and (k is None or n < k):
                        try:
                            next(bgq[0])
                            n += 1
                        except StopIteration:
                            bgq.pop(0)

                def routing_gen(t0, par):
                    for j in range(NJ):
                        T = t0 + j
                        sj = par * 2 + j
                        Sbj = Sb[sj]
                        SbK = [("Sb", sj, n) for n in range(4)]
                        for i0 in range(0, 16, 4):
                            for i in range(i0, i0 + 4):
                                DVE(lambda e, i=i, j=j, sj=sj: e.max(out=Vt[j][:, i, 0:8], in_=Sb[sj][:, i * P:(i + 1) * P]),
                                    r=[("Sb", sj, i // 4)], w=[("Vt", j, i, 0)])
                            yield
                        for i0 in range(0, 16, 4):
                            for i in range(i0, i0 + 4):
                                DVE(lambda e, i=i, j=j, sj=sj: e.max_index(out=It[j][:, i, 0:8], in_max=Vt[j][:, i, 0:8],
                                                                    in_values=Sb[sj][:, i * P:(i + 1) * P]),
                                    r=[("Sb", sj, i // 4), ("Vt", j, i, 0)], w=[("It", j, i, 0)])
                            yield
                        for i0 in range(0, 16, 4):
                            for i in range(i0, i0 + 4):
                                DVE(lambda e, i=i, j=j, sj=sj: e.match_replace(out=S2x[:, i * P:(i + 1) * P], in_to_replace=Vt[j][:, i, 0:8],
                                                                        in_values=Sb[sj][:, i * P:(i + 1) * P], imm_value=NEG),
                                    r=[("Sb", sj, i // 4), ("Vt", j, i, 0)], w=[("S2", i)])
                            yield
                        for i0 in range(0, 16, 4):
                            for i in range(i0, i0 + 4):
                                DVE(lambda e, i=i, j=j, sj=sj: e.max(out=Vt[j][:, i, 8:16], in_=S2x[:, i * P:(i + 1) * P]),
                                    r=[("S2", i)], w=[("Vt", j, i, 1)])
                            yield
                        for i0 in range(0, 16, 4):
                            for i in range(i0, i0 + 4):
                                DVE(lambda e, i=i, j=j, sj=sj: e.max_index(out=It[j][:, i, 8:16], in_max=Vt[j][:, i, 8:16],
                                                                    in_values=S2x[:, i * P:(i + 1) * P]),
                                    r=[("S2", i), ("Vt", j, i, 1)], w=[("It", j, i, 1)])
                            yield
                        allV = [("Vt", j, i, k) for i in range(16) for k in range(2)]
                        allI = [("It", j, i, k) for i in range(16) for k in range(2)]
                        DVE(lambda e, j=j: e.tensor_copy(out=Itf[j][:], in_=It[j][:]), r=allI, w=[("Itf", j)])
                        cand = Sbj[:].rearrange("p (h c) -> p h c", h=8)
                        cand2 = S2x[:].rearrange("p (h c) -> p h c", h=8)
                        Vv = Vt[j][:].rearrange("p (h f) k -> p h f k", f=2)
                        DVE(lambda e, cand=cand, Vv=Vv: e.tensor_tensor(
                            out=cand.rearrange("p h (a b) -> p h a b", a=16),
                            in0=Vv[:, :, 0, :].unsqueeze(3).to_broadcast([P, 8, 16, 16]),
                            in1=Vv[:, :, 1, :].unsqueeze(2).to_broadcast([P, 8, 16, 16]), op=ALU.add),
                            r=allV + SbK, w=SbK)
                        yield
                        for h0 in range(0, 8, 4):
                            for h in range(h0, h0 + 4):
                                DVE(lambda e, h=h, j=j, cand=cand: e.max(out=Bt[j][:, h, 0:8], in_=cand[:, h, :]),
                                    r=SbK, w=[("Bt", j, h, 0)])
                            yield
                        for h0 in range(0, 8, 4):
                            for h in range(h0, h0 + 4):
                                DVE(lambda e, h=h, j=j, cand=cand: e.max_index(out=CI[j][:, h, 0:8], in_max=Bt[j][:, h, 0:8],
                                                                               in_values=cand[:, h, :]),
                                    r=SbK + [("Bt", j, h, 0)], w=[("CI", j, h, 0)])
                            yield
                        for h0 in range(0, 8, 4):
                            for h in range(h0, h0 + 4):
                                DVE(lambda e, h=h, j=j, cand=cand, cand2=cand2: e.match_replace(
                                    out=cand2[:, h, :], in_to_replace=Bt[j][:, h, 0:8], in_values=cand[:, h, :], imm_value=NEG),
                                    r=SbK + [("Bt", j, h, 0)], w=[("S2", 2 * h), ("S2", 2 * h + 1)])
                            yield
                        for h0 in range(0, 8, 4):
                            for h in range(h0, h0 + 4):
                                DVE(lambda e, h=h, j=j, cand2=cand2: e.max(out=Bt[j][:, h, 8:16], in_=cand2[:, h, :]),
                                    r=[("S2", 2 * h), ("S2", 2 * h + 1)], w=[("Bt", j, h, 1)])
                            yield
                        for h0 in range(0, 8, 4):
                            for h in range(h0, h0 + 4):
                                DVE(lambda e, h=h, j=j, cand2=cand2: e.max_index(out=CI[j][:, h, 8:16], in_max=Bt[j][:, h, 8:16],
                                                                                in_values=cand2[:, h, :]),
                                    r=[("S2", 2 * h), ("S2", 2 * h + 1), ("Bt", j, h, 1)], w=[("CI", j, h, 1)])
                            yield
                        allB = [("Bt", j, h, k) for h in range(8) for k in range(2)]
                        allC = [("CI", j, h, k) for h in range(8) for k in range(2)]
                        POOL(lambda e, j=j: e.tensor_tensor(
                            out=exg[j][:].rearrange("p (h k) -> p h k", h=8), in0=Bt[j][:],
                            in1=Bt[j][:, :, 0:1].to_broadcast([P, 8, 16]), op=ALU.subtract),
                            r=allB, w=[("exg", j)])
                        ACT(lambda e, j=j: e.activation(out=exg[j][:], in_=exg[j][:], func=AF.Exp),
                            r=[("exg", j)], w=[("exg", j)])
                        DVE(lambda e, j=j: e.tensor_single_scalar(out=CIu[j][:, 0, :], in_=CI[j][:].rearrange("p h k -> p (h k)"),
                                                                  scalar=15, op=ALU.bitwise_and),
                            r=allC, w=[("CIu", j, 0)])
                        DVE(lambda e, j=j: e.tensor_single_scalar(out=CIu[j][:, 1, :], in_=CI[j][:].rearrange("p h k -> p (h k)"),
                                                                  scalar=4, op=ALU.logical_shift_right),
                            r=allC, w=[("CIu", j, 1)])
                        DVE(lambda e, j=j: e.tensor_copy(out=CIf[j][:, 1, :], in_=CIu[j][:, 0, :]),
                            r=[("CIu", j, 0)], w=[("CIf", j, 1)])
                        DVE(lambda e, j=j: e.tensor_copy(out=CIf[j][:, 2, :], in_=CIu[j][:, 1, :]),
                            r=[("CIu", j, 1)], w=[("CIf", j, 2)])
                        yield
                        Iv = Itf[j][:].rearrange("p (h f) k -> p h f k", f=2)
                        specs = ((0, 2, 0, Sbj, SbK), (1, 1, 1, S2x, [("S2", i) for i in range(16)]))
                        for which, row, half, mbuf, mkeys in specs:
                            mk = mbuf[:].rearrange("p (h q a) -> p h q a", h=8, q=16)
                            DVE(lambda e, j=j, row=row, mk=mk: e.tensor_tensor(
                                out=mk, in0=iota16[:].unsqueeze(1).unsqueeze(1).to_broadcast([P, 8, 16, 16]),
                                in1=CIf[j][:, row, :].rearrange("p (h q) -> p h q", h=8).unsqueeze(3).to_broadcast([P, 8, 16, 16]),
                                op=ALU.is_equal),
                                r=[("CIf", j, row), "iota16"] + mkeys, w=mkeys)
                            POOL(lambda e, half=half, mk=mk, Iv=Iv: e.tensor_tensor(
                                out=mk, in0=mk, in1=Iv[:, :, half, :].unsqueeze(2).to_broadcast([P, 8, 16, 16]), op=ALU.mult),
                                r=mkeys + [("Itf", j)], w=mkeys)
                            yield
                        yield
                        for which, row, half, mbuf, mkeys in specs:
                            mk = mbuf[:].rearrange("p (h q a) -> p h q a", h=8, q=16)
                            DVE(lambda e, j=j, which=which, mk=mk: e.tensor_reduce(
                                out=JB[j][:, which, :].rearrange("p (h q) -> p h q", h=8), in_=mk, axis=AX.X, op=ALU.add),
                                r=mkeys, w=[("JB", j, which)])
                        yield
                        DVE(lambda e, j=j: e.tensor_reduce(out=zs[j][:, 0:8], in_=exg[j][:].rearrange("p (h k) -> p h k", h=8),
                                                           axis=AX.X, op=ALU.add),
                            r=[("exg", j)], w=[("zs", j)])
                        DVE(lambda e, j=j: e.reciprocal(out=zs[j][:, 8:16], in_=zs[j][:, 0:8]), r=[("zs", j)], w=[("zr", j)])
                        POOL(lambda e, j=j: e.tensor_tensor(
                            out=JB[j][:, 2, :].rearrange("p (h k) -> p h k", h=8),
                            in0=exg[j][:].rearrange("p (h k) -> p h k", h=8),
                            in1=zs[j][:, 8:16].unsqueeze(2).to_broadcast([P, 8, 16]), op=ALU.mult),
                            r=[("exg", j), ("zr", j)], w=[("JB", j, 2)])
                        yield
                        bk = next_bank()
                        for w3 in range(3):
                            PE(lambda e, w3=w3, j=j, bk=bk: e.transpose(out=banks[bk][:, w3 * P:(w3 + 1) * P], in_=JB[j][:, w3, :],
                                                                       identity=ident_f[:]),
                               r=[("JB", j, w3), "ident_f"], w=[("ps", bk)])
                        ACT(lambda e, j=j, bk=bk: e.activation(out=JT[j][:].rearrange("p w t -> p (w t)"), in_=banks[bk][:, 0:3 * P],
                                                               func=AF.Copy),
                            r=[("ps", bk)], w=[("JT", j)])
                        DMA(lambda e, T=T, j=j: e.dma_start(out=jt_scr[T * P:(T + 1) * P, :], in_=JT[j][:].rearrange("p w t -> p (w t)")),
                            r=[("JT", j)], w=["jt_scr"])
                        yield

                xslot = [0]
                for g in range(NG):
                    t0 = g * NJ
                    bA, bB = next_bank(), next_bank()
                    for j in range(NJ):
                        T = t0 + j
                        sl = xslot[0] % 2
                        xslot[0] += 1
                        DMA(lambda e, T=T, sl=sl: e.dma_start(out=xbuf[sl][:], in_=x[T * P:(T + 1) * P, :]), w=[("xbuf", sl)])
                        tick(3)
                        rms_tile(xbuf[sl], ("xbuf", sl), xn[sl], ("xn", sl), ms[sl], "ms%d" % sl)
                        transpose_tile(xn[sl], ("xn", sl), bA, bB, j, GT)
                    tick(3)
                    evac_T(nT, "nT", bA, bB, G1, GT)
                    DVE(lambda e: e.tensor_copy(out=pace[:, 0:1], in_=epsT[:, 0:1]), r=["epsT"], w=[("pace", g)])
                    nb_ = P // NG
                    cast_experts(g * nb_, (g + 1) * nb_, dep=[("pace", g)])

                    for n in range(14):
                        slot = load_wb(g * NWB_G + n)
                        tick(4)
                        if n in (0, 1, 4, 5):
                            for j in range(NJ):
                                T = t0 + j
                                bk = next_bank()
                                for c in range(8):
                                    PE(lambda e, c=c, j=j, bk=bk, slot=slot: e.matmul(
                                        out=banks[bk][:], lhsT=nT[:, c, j * P:(j + 1) * P], rhs=wb[slot][:, c, :],
                                        start=(c == 0), stop=(c == 7)),
                                       r=["nT", ("wb", slot)], w=[("ps", bk)])
                                if n < 2:
                                    ACT(lambda e, T=T, n=n, bk=bk: e.activation(
                                        out=Pr[T % 4][:, n * 512:(n + 1) * 512], in_=banks[bk][:], func=AF.Copy),
                                        r=[("ps", bk)], w=[("Pr", T % 4)])
                                else:
                                    nn = n - 4
                                    ACT(lambda e, j=j, nn=nn, bk=bk: e.activation(
                                        out=gv[j][:, nn * 512:(nn + 1) * 512], in_=banks[bk][:], func=AF.Gelu,
                                        accum_out=lst[j][:, nn:nn + 1]),
                                        r=[("ps", bk)], w=[("gv", j), ("lst", j, nn)])
                        else:
                            for kk in range(2):
                                bk = next_bank()
                                for k2 in range(2):
                                    k = kk * 2 + k2
                                    for c in range(8):
                                        PE(lambda e, c=c, k=k, k2=k2, bk=bk, slot=slot: e.matmul(
                                            out=banks[bk][:, k2 * GT:(k2 + 1) * GT], lhsT=wb[slot][:, c, k * P:(k + 1) * P],
                                            rhs=nT[:, c, :], start=(c == 0), stop=(c == 7)),
                                           r=["nT", ("wb", slot)], w=[("ps", bk)])
                                src = banks[bk][:].rearrange("p (a t) -> p a t", a=2)
                                if n in (2, 3):
                                    ch = (n - 2) * 4 + kk * 2
                                    ACT(lambda e, ch=ch, src=src: e.activation(out=uT[:, ch:ch + 2, :], in_=src, func=AF.Gelu),
                                        r=[("ps", bk)], w=["uT"])
                                elif n in (6, 7):
                                    ch = (n - 6) * 4 + kk * 2
                                    ACT(lambda e, ch=ch, src=src: e.activation(out=qT[:, ch:ch + 2, :], in_=src, func=AF.Copy),
                                        r=[("ps", bk)], w=["qT"])
                                else:
                                    ch = (n - 8) * 4 + kk * 2
                                    ACT(lambda e, ch=ch, src=src: e.activation(out=gT[:, ch:ch + 2, :], in_=src, func=AF.Sigmoid),
                                        r=[("ps", bk)], w=["gT"])
                        if n == 5:
                            for j in range(NJ):
                                ACT(lambda e, j=j: e.activation(out=zt[j][:], in_=gv[j][:], func=AF.Square,
                                                                accum_out=lst[j][:, 2:3]),
                                    r=[("gv", j)], w=[("zt", j), ("lst", j, 2)])
                                tick(1)
                                DVE(lambda e, j=j: e.tensor_scalar(out=lst[j][:, 3:4], in0=lst[j][:, 0:1], scalar1=lst[j][:, 1:2],
                                                                   scalar2=1.0 / D, op0=ALU.add, op1=ALU.mult),
                                    r=[("lst", j, 0), ("lst", j, 1)], w=[("lst", j, 3)])
                                DVE(lambda e, j=j: e.tensor_tensor(out=lst[j][:, 4:5], in0=lst[j][:, 3:4], in1=lst[j][:, 3:4],
                                                                   op=ALU.mult),
                                    r=[("lst", j, 3)], w=[("lst", j, 4)])
                                DVE(lambda e, j=j: e.scalar_tensor_tensor(out=lst[j][:, 5:6], in0=lst[j][:, 2:3], scalar=1.0 / D,
                                                                          in1=lst[j][:, 4:5], op0=ALU.mult, op1=ALU.subtract),
                                    r=[("lst", j, 2), ("lst", j, 4)], w=[("lst", j, 5)])
                                ACT(lambda e, j=j: e.activation(out=lst[j][:, 7:8], in_=lst[j][:, 5:6], func=AF.Sqrt,
                                                                bias=epsT[:, 1:2], scale=1.0),
                                    r=[("lst", j, 5), "epsT"], w=[("lst", j, 7)])
                                DVE(lambda e, j=j: e.reciprocal(out=lst[j][:, 6:7], in_=lst[j][:, 7:8]),
                                    r=[("lst", j, 7)], w=[("lst", j, 6)])
                                DVE(lambda e, j=j: e.tensor_scalar(out=zt[j][:], in0=gv[j][:], scalar1=lst[j][:, 3:4],
                                                                   scalar2=lst[j][:, 6:7], op0=ALU.subtract, op1=ALU.mult),
                                    r=[("gv", j), ("lst", j, 3), ("lst", j, 6)], w=[("zt", j)])

                    for j in range(NJ):
                        T = t0 + j
                        for half in range(2):
                            bk = next_bank()
                            for cc in range(4):
                                c = half * 4 + cc
                                gi = c // 2
                                band0 = (8 + gi) if T == 0 else gi
                                PE(lambda e, c=c, cc=cc, T=T, bk=bk, band0=band0: e.matmul(
                                    out=banks[bk][:, cc * P:(cc + 1) * P], lhsT=Pr[T % 4][:, c * P:(c + 1) * P],
                                    rhs=bands[:, band0, :], start=True, stop=(T == 0)),
                                   r=[("Pr", T % 4), "bands"], w=[("ps", bk)])
                                if T > 0:
                                    PE(lambda e, c=c, cc=cc, T=T, bk=bk, gi=gi: e.matmul(
                                        out=banks[bk][:, cc * P:(cc + 1) * P], lhsT=Pr[(T - 1) % 4][:, c * P:(c + 1) * P],
                                        rhs=bands[:, 4 + gi, :], start=False, stop=True),
                                       r=[("Pr", (T - 1) % 4), "bands"], w=[("ps", bk)])
                            ACT(lambda e, half=half, j=j, bk=bk: e.activation(
                                out=diffT[:, half * 4:half * 4 + 4, j * P:(j + 1) * P],
                                in_=banks[bk][:].rearrange("p (c t) -> p c t", c=4), func=AF.Copy),
                                r=[("ps", bk)], w=["diffT"])
                    for dc in range(8):
                        if dc % 2 == 0:
                            bk = next_bank()
                        gi = dc // 2
                        for kc in range(2):
                            PE(lambda e, dc=dc, gi=gi, kc=kc, bk=bk: e.matmul(
                                out=banks[bk][:, (dc % 2) * GT:(dc % 2 + 1) * GT],
                                lhsT=pw_t[:, gi * 2 + kc, (dc % 2) * P:(dc % 2 + 1) * P], rhs=diffT[:, gi * 2 + kc, :],
                                start=(kc == 0), stop=(kc == 1)),
                               r=["pw", "diffT"], w=[("ps", bk)])
                        tick(1)
                        DVE(lambda e, dc=dc, bk=bk: e.scalar_tensor_tensor(
                            out=M32[:, dc, :], in0=banks[bk][:, (dc % 2) * GT:(dc % 2 + 1) * GT],
                            scalar=vec_t[:, PSC, dc:dc + 1], in1=gT[:, dc, :], op0=ALU.mult, op1=ALU.mult),
                            r=[("ps", bk), "vec", "gT"], w=["M32"])

                    for j in range(NJ):
                        for half in range(2):
                            bk = next_bank()
                            for hh in range(4):
                                h = half * 4 + hh
                                PE(lambda e, h=h, hh=hh, j=j, bk=bk: e.matmul(
                                    out=banks[bk][:, hh * P:(hh + 1) * P], lhsT=zt[j][:, h * P:(h + 1) * P],
                                    rhs=WmT[:, h, :], start=True, stop=True),
                                   r=[("zt", j), "WmT"], w=[("ps", bk)])
                            h0 = half * 4
                            tick(1)
                            DVE(lambda e, h0=h0, bk=bk: e.tensor_tensor(
                                out=tmpS[:], in0=banks[bk][:].rearrange("p (h t) -> p h t", h=4),
                                in1=vec_t[:, LNG, h0:h0 + 4].unsqueeze(2).to_broadcast([P, 4, P]), op=ALU.mult),
                                r=[("ps", bk), "vec"], w=["tmpS"])
                            DVE(lambda e, h0=h0: e.tensor_tensor(out=tmpS[:], in0=tmpS[:], in1=Cm[:, h0:h0 + 4, :], op=ALU.add),
                                r=["tmpS", "Cm"], w=["tmpS"])
                            DVE(lambda e, h0=h0, j=j: e.tensor_tensor(
                                out=gatedT[:, h0:h0 + 4, j * P:(j + 1) * P], in0=tmpS[:],
                                in1=uT[:, h0:h0 + 4, j * P:(j + 1) * P], op=ALU.mult),
                                r=["tmpS", "uT"], w=["gatedT"])
                    for dcp in range(4):
                        if dcp % 2 == 0:
                            slot = load_wb(g * NWB_G + 14 + dcp // 2)
                        tick(2)
                        bk = next_bank()
                        for d2 in range(2):
                            dc = dcp * 2 + d2
                            for hc in range(8):
                                PE(lambda e, dc=dc, d2=d2, hc=hc, bk=bk, slot=slot: e.matmul(
                                    out=banks[bk][:, d2 * GT:(d2 + 1) * GT], lhsT=wb[slot][:, hc, (dc % 4) * P:(dc % 4 + 1) * P],
                                    rhs=gatedT[:, hc, :], start=(hc == 0), stop=(hc == 7)),
                                   r=[("wb", slot), "gatedT"], w=[("ps", bk)])
                        dc0 = dcp * 2
                        tick(1)
                        DVE(lambda e, dc0=dc0, bk=bk: e.tensor_tensor(
                            out=tmpM[:], in0=banks[bk][:].rearrange("p (a t) -> p a t", a=2),
                            in1=gT[:, 8 + dc0:8 + dc0 + 2, :], op=ALU.mult),
                            r=[("ps", bk), "gT"], w=["tmpM"])
                        DVE(lambda e, dc0=dc0: e.tensor_tensor(out=M32[:, dc0:dc0 + 2, :], in0=M32[:, dc0:dc0 + 2, :],
                                                               in1=tmpM[:], op=ALU.add),
                            r=["tmpM", "M32"], w=["M32"])

                    for j in range(NJ):
                        bks = [next_bank(), next_bank()]
                        for a in range(4):
                            bk = bks[a // 2]
                            for kc in range(2):
                                PE(lambda e, a=a, kc=kc, j=j, bk=bk: e.matmul(
                                    out=banks[bk][:, (a % 2) * NMEM:(a % 2 + 1) * NMEM],
                                    lhsT=qT[:, 2 * a + kc, j * P:(j + 1) * P], rhs=kT[:, 2 * a + kc, :],
                                    start=(kc == 0), stop=(kc == 1)),
                                   r=["qT", "kT"], w=[("ps", bk)])
                        for hb_ in range(2):
                            bk = bks[hb_]
                            tick(1)
                            DVE(lambda e, j=j, hb_=hb_, bk=bk: e.tensor_reduce(
                                out=smx[j][:, hb_ * 2:hb_ * 2 + 2], in_=banks[bk][:].rearrange("p (a m) -> p a m", a=2),
                                axis=AX.X, op=ALU.max),
                                r=[("ps", bk)], w=[("smx", j, hb_)])
                            DVE(lambda e, j=j, hb_=hb_: e.tensor_scalar(
                                out=smx[j][:, 4 + hb_ * 2:4 + hb_ * 2 + 2], in0=smx[j][:, hb_ * 2:hb_ * 2 + 2],
                                scalar1=-1.0 / 16.0, scalar2=None, op0=ALU.mult),
                                r=[("smx", j, hb_)], w=[("smxn", j, hb_)])
                        for a in range(4):
                            bk = bks[a // 2]
                            ACT(lambda e, a=a, j=j, bk=bk: e.activation(
                                out=ex[j][:, a, :], in_=banks[bk][:, (a % 2) * NMEM:(a % 2 + 1) * NMEM], func=AF.Exp,
                                bias=smx[j][:, 4 + a:5 + a], scale=1.0 / 16.0, accum_out=smx[j][:, 8 + a:9 + a]),
                                r=[("ps", bk), ("smxn", j, a // 2)], w=[("ex", j), ("sse", j, a)])
                        tick(1)
                        DVE(lambda e, j=j: e.reciprocal(out=smx[j][:, 12:16], in_=smx[j][:, 8:12]),
                            r=[("sse", j, a) for a in range(4)], w=[("srs", j)])
                        DVE(lambda e, j=j: e.tensor_tensor(
                            out=ex[j][:], in0=ex[j][:], in1=smx[j][:, 12:16].unsqueeze(2).to_broadcast([P, 4, NMEM]),
                            op=ALU.mult),
                            r=[("ex", j), ("srs", j)], w=[("ex", j)])
                        bk = next_bank()
                        psb = banks[bk][:].bitcast(BF16)
                        for a in range(4):
                            for mc in range(2):
                                o = (a * 2 + mc) * P
                                PE(lambda e, a=a, mc=mc, o=o, j=j, psb=psb: e.transpose(
                                    out=psb[:, o:o + P], in_=ex[j][:, a, mc * P:(mc + 1) * P], identity=ident[:]),
                                   r=[("ex", j), "ident"], w=[("ps", bk)])
                        ACT(lambda e, j=j, psb=psb: e.activation(
                            out=probsT[:, :, j * P:(j + 1) * P], in_=psb[:].rearrange("p (c t) -> p c t", c=8), func=AF.Copy),
                            r=[("ps", bk)], w=["probsT"])
                    for hcp in range(4):
                        bk = next_bank()
                        for h2 in range(2):
                            hc = hcp * 2 + h2
                            a = hc // 2
                            for mc in range(2):
                                PE(lambda e, hc=hc, h2=h2, a=a, mc=mc, bk=bk: e.matmul(
                                    out=banks[bk][:, h2 * GT:(h2 + 1) * GT], lhsT=vmem[:, mc, hc * P:(hc + 1) * P],
                                    rhs=probsT[:, a * 2 + mc, :], start=(mc == 0), stop=(mc == 1)),
                                   r=["vmem", "probsT"], w=[("ps", bk)])
                        ACT(lambda e, hcp=hcp, bk=bk: e.activation(
                            out=oT[:, hcp * 2:hcp * 2 + 2, :], in_=banks[bk][:].rearrange("p (a t) -> p a t", a=2), func=AF.Copy),
                            r=[("ps", bk)], w=["oT"])
                    for dcp in range(4):
                        if dcp % 2 == 0:
                            slot = load_wb(g * NWB_G + 16 + dcp // 2)
                        tick(2)
                        bk = next_bank()
                        for d2 in range(2):
                            dc = dcp * 2 + d2
                            for hc in range(8):
                                PE(lambda e, dc=dc, d2=d2, hc=hc, bk=bk, slot=slot: e.matmul(
                                    out=banks[bk][:, d2 * GT:(d2 + 1) * GT], lhsT=wb[slot][:, hc, (dc % 4) * P:(dc % 4 + 1) * P],
                                    rhs=oT[:, hc, :], start=(hc == 0), stop=(hc == 7)),
                                   r=[("wb", slot), "oT"], w=[("ps", bk)])
                        dc0 = dcp * 2
                        tick(1)
                        DVE(lambda e, dc0=dc0, bk=bk: e.tensor_tensor(
                            out=tmpM[:], in0=banks[bk][:].rearrange("p (a t) -> p a t", a=2),
                            in1=gT[:, 16 + dc0:16 + dc0 + 2, :], op=ALU.mult),
                            r=[("ps", bk), "gT"], w=["tmpM"])
                        DVE(lambda e, dc0=dc0: e.tensor_tensor(out=Mb[:, dc0:dc0 + 2, :], in0=M32[:, dc0:dc0 + 2, :],
                                                               in1=tmpM[:], op=ALU.add),
                            r=["tmpM", "M32"], w=["Mb"])

                    xsl = []
                    for j in range(NJ):
                        T = t0 + j
                        sl = xslot[0] % 2
                        xslot[0] += 1
                        xsl.append(sl)
                        DMA(lambda e, T=T, sl=sl: e.dma_start(out=xbuf[sl][:], in_=x[T * P:(T + 1) * P, :]), w=[("xbuf", sl)])
                    for n in range(2):
                        slot = load_wb(g * NWB_G + 18 + n)
                        tick(2)
                        for j in range(NJ):
                            sl = xsl[j]
                            bk = next_bank()
                            for c in range(8):
                                PE(lambda e, c=c, j=j, bk=bk, slot=slot: e.matmul(
                                    out=banks[bk][:], lhsT=Mb[:, c, j * P:(j + 1) * P], rhs=wb[slot][:, c, :],
                                    start=(c == 0), stop=(c == 7)),
                                   r=["Mb", ("wb", slot)], w=[("ps", bk)])
                            tick(1)
                            DVE(lambda e, j=j, n=n, bk=bk, sl=sl: e.tensor_tensor(
                                out=gv[j][:, n * 512:(n + 1) * 512], in0=banks[bk][:], in1=xbuf[sl][:, n * 512:(n + 1) * 512],
                                op=ALU.add),
                                r=[("ps", bk), ("xbuf", sl)], w=[("gv", j)])
                    for j in range(NJ):
                        T = t0 + j
                        DMA(lambda e, T=T, j=j: e.dma_start(out=h_scr[T * P:(T + 1) * P, :], in_=gv[j][:]),
                            r=[("gv", j)], w=["h_scr"])
                    sp_ = g % 2
                    while len(bgq) > 1:
                        for _ in bgq[0]:
                            pass
                        bgq.pop(0)
                    bA, bB = next_bank(), next_bank()
                    for j in range(NJ):
                        rms_tile(gv[j], ("gv", j), xn[j], ("xn", j), ms[j], "ms%d" % j)
                        transpose_tile(xn[j], ("xn", j), bA, bB, j, GT)
                    evac_T(n2T, "n2T", bA, bB, G2, GT)
                    DMA(lambda e, g=g: e.dma_start(out=n2t_v[g], in_=n2T[:].rearrange("p c t -> p (c t)")),
                        r=["n2T"], w=["n2t_scr"])
                    for n in range(4):
                        slot = load_wb(g * NWB_G + 20 + n)
                        for j in range(NJ):
                            bk = next_bank()
                            for c in range(8):
                                PE(lambda e, c=c, j=j, bk=bk, slot=slot: e.matmul(
                                    out=banks[bk][:], lhsT=n2T[:, c, j * P:(j + 1) * P], rhs=wb[slot][:, c, :],
                                    start=(c == 0), stop=(c == 7)),
                                   r=["n2T", ("wb", slot)], w=[("ps", bk)])
                            ACT(lambda e, j=j, n=n, bk=bk, sp_=sp_: e.activation(out=Sb[sp_ * 2 + j][:, n * 512:(n + 1) * 512],
                                                                                 in_=banks[bk][:], func=AF.Copy),
                                r=[("ps", bk)], w=[("Sb", sp_ * 2 + j, n)])
                    bgq.append(routing_gen(t0, sp_))
                tick(None)
            sch.barrier()
        else:
            cast_experts()

        if "B2" in phases:
            esC = ExitStack()
            with esC:
                sbC = lambda name, shape, dt: esC.enter_context(nc.sbuf_tensor(name, list(shape), dt))
                iotaE = sbC("iotaE", [P, P], BF16)
                DMA(lambda e: e.dma_start(out=iotaE[:], in_=c_iota[:, :]), w=["iotaE"], q="pool")
                hbC = sbC("hbC", [P, D], F32)
                n2TC = [sbC("n2TC%d" % i, [P, 8, GT], BF16) for i in range(2)]
                JTC = [sbC("JTC%d" % i, [P, 3, P], F32) for i in range(4)]
                TP = 8
                NOH = 2
                OH1s = [sbC("OH1s%d" % i, [P, TP, P], BF16) for i in range(NOH)]
                OH2s = [sbC("OH2s%d" % i, [P, TP, P], BF16) for i in range(NOH)]
                Gf = [sbC("Gf%d" % i, [P, GT, P], BF16) for i in range(2)]
                NUB, NVB = 3, 4
                ub = [sbC("ub%d" % i, [P, 2, 8, P], BF16) for i in range(NUB)]
                vb = [sbC("vb%d" % i, [P, 2, D], BF16) for i in range(NVB)]
                NHA = 3
                Hg = [sbC("Hg%d" % i, [P, 2, GT], BF16) for i in range(NHA)]
                At = [sbC("At%d" % i, [P, 2, GT], BF16) for i in range(NHA)]
                ms3 = sbC("ms3", [P, 4], F32)
                ob = sbC("ob", [P, D], F32)

                n2t_v = n2t_scr.rearrange("(g p) f -> g p f", p=P)
                ut_v = ut_bf.rearrange("(b p) (c e) -> p b c e", p=P, c=8)
                v_v = v_bf.rearrange("(b e) d -> e b d", e=P)
                YB = [[0, 1], [2, 3]]
                HB = [4, 5]
                MB = [6, 7]
                NP2 = P // 2
                uq = [0]
                vq = [0]
                ohc = [0]

                def load_group_inputs(gg):
                    par = gg % 2
                    DMA(lambda e, gg=gg, par=par: e.dma_start(out=n2TC[par][:].rearrange("p c t -> p (c t)"), in_=n2t_v[gg]),
                        r=["n2t_scr"], w=[("n2TC", par)])
                    for j in range(NJ):
                        T = gg * NJ + j
                        DMA(lambda e, T=T, j=j, par=par: e.dma_start(out=JTC[par * 2 + j][:].rearrange("p w t -> p (w t)"),
                                                                     in_=jt_scr[T * P:(T + 1) * P, :]),
                            r=["jt_scr"], w=[("JTC", par * 2 + j)])

                def prefetch_u(hi):
                    while uq[0] <= min(hi, NG * NP2 - 1):
                        idx = uq[0]
                        uq[0] += 1
                        b0 = (idx % NP2) * 2
                        DMA(lambda e, b0=b0, us=idx % NUB: e.dma_start(out=ub[us][:], in_=ut_v[:, b0:b0 + 2, :, :]),
                            r=["ut_bf"], w=[("ub", idx % NUB)])

                def prefetch_v(hi):
                    while vq[0] <= min(hi, NG * NP2 - 1):
                        idx = vq[0]
                        vq[0] += 1
                        b0 = (idx % NP2) * 2
                        DMA(lambda e, b0=b0, vs=idx % NVB: e.dma_start(out=vb[vs][:], in_=v_v[:, b0:b0 + 2, :]),
                            r=["v_bf"], w=[("vb", idx % NVB)])

                def build_steps(gg):
                    par = gg % 2
                    Gd = Gf[par]
                    deferred = [None]
                    for j in range(NJ):
                        jt = JTC[par * 2 + j]
                        jk = ("JTC", par * 2 + j)
                        for pc in range(P // TP):
                            sl = ohc[0] % NOH
                            ohc[0] += 1
                            tq = pc * TP
                            iob = iotaE[:].unsqueeze(1).to_broadcast([P, TP, P])
                            DVE(lambda e, sl=sl, tq=tq, iob=iob, jt=jt: e.tensor_tensor(
                                out=OH1s[sl][:], in0=iob, in1=jt[:, 0, tq:tq + TP].unsqueeze(2).to_broadcast([P, TP, P]),
                                op=ALU.is_equal),
                                r=["iotaE", jk], w=[("OH1", sl)])
                            DVE(lambda e, sl=sl, tq=tq, iob=iob, jt=jt: e.tensor_tensor(
                                out=OH2s[sl][:], in0=iob, in1=jt[:, 1, tq:tq + TP].unsqueeze(2).to_broadcast([P, TP, P]),
                                op=ALU.is_equal),
                                r=["iotaE", jk], w=[("OH2", sl)])
                            POOL(lambda e, sl=sl, tq=tq, jt=jt: e.tensor_tensor(
                                out=OH2s[sl][:], in0=OH2s[sl][:], in1=jt[:, 2, tq:tq + TP].unsqueeze(2).to_broadcast([P, TP, P]),
                                op=ALU.mult),
                                r=[("OH2", sl), jk], w=[("OH2", sl)])
                            def pe_part(sl=sl, j=j, tq=tq, Gd=Gd, par=par):
                                for q4 in range(TP // 4):
                                    bk = next_bank(MB)
                                    for tt in range(4):
                                        tl = q4 * 4 + tt
                                        PE(lambda e, sl=sl, tl=tl, tt=tt, bk=bk: e.matmul(
                                            out=banks[bk][:, tt * P:(tt + 1) * P], lhsT=OH2s[sl][:, tl, :], rhs=OH1s[sl][:, tl, :],
                                            start=True, stop=True),
                                           r=[("OH1", sl), ("OH2", sl)], w=[("ps", bk)])
                                    tok = j * P + tq + q4 * 4
                                    ACT(lambda e, tok=tok, bk=bk, Gd=Gd: e.activation(
                                        out=Gd[:, tok:tok + 4, :], in_=banks[bk][:].rearrange("p (t e) -> p t e", t=4), func=AF.Copy),
                                        r=[("ps", bk)], w=[("Gf", par)])
                            if deferred[0] is not None:
                                deferred[0]()
                            deferred[0] = pe_part
                            yield
                    if deferred[0] is not None:
                        deferred[0]()
                        deferred[0] = None
                load_group_inputs(0)
                prefetch_u(1)
                prefetch_v(1)
                for _ in build_steps(0):
                    pass
                LAG = 2
                for g in range(NG):
                    t0 = g * NJ
                    par = g % 2
                    bg = None
                    if g + 1 < NG:
                        load_group_inputs(g + 1)
                        bg = build_steps(g + 1)
                    nsteps = NJ * (P // TP)
                    done_steps = 0
                    pend = []
                    for bp in range(NP2 + LAG):
                        gq = g * NP2 + bp
                        if bp < NP2:
                            b0 = bp * 2
                            prefetch_u(gq)
                            us = gq % NUB
                            ha = bp % NHA
                            hbk = HB[bp % 2]
                            for k in range(2):
                                for c in range(8):
                                    PE(lambda e, us=us, c=c, k=k, hbk=hbk, par=par: e.matmul(
                                        out=banks[hbk][:, k * GT:(k + 1) * GT], lhsT=ub[us][:, k, c, :], rhs=n2TC[par][:, c, :],
                                        start=(c == 0), stop=(c == 7)),
                                       r=[("ub", us), ("n2TC", par)], w=[("ps", hbk)])
                            ACT(lambda e, ha=ha, hbk=hbk: e.activation(
                                out=Hg[ha][:], in_=banks[hbk][:].rearrange("p (k t) -> p k t", k=2), func=AF.Gelu),
                                r=[("ps", hbk)], w=[("Hg", ha)])
                            gsrc = Gf[par][:, :, b0:b0 + 2].rearrange("p t k -> p k t")
                            DVE(lambda e, ha=ha, gsrc=gsrc: e.tensor_tensor(out=At[ha][:], in0=Hg[ha][:], in1=gsrc, op=ALU.mult),
                                r=[("Hg", ha), ("Gf", par)], w=[("At", ha)])
                            pend.append((ha, gq, b0))
                        if bp >= LAG:
                            pha, pgq, pb0 = pend.pop(0)
                            prefetch_v(pgq)
                            pvs = pgq % NVB
                            for k in range(2):
                                b = pb0 + k
                                for j in range(NJ):
                                    for n in range(2):
                                        ybk = YB[j][n]
                                        PE(lambda e, pha=pha, pvs=pvs, k=k, j=j, n=n, ybk=ybk, b=b: e.matmul(
                                            out=banks[ybk][:], lhsT=At[pha][:, k, j * P:(j + 1) * P],
                                            rhs=vb[pvs][:, k, n * 512:(n + 1) * 512], start=(b == 0), stop=(b == P - 1)),
                                           r=[("At", pha), ("vb", pvs)], w=[("ps", ybk)])
                            prefetch_v(pgq + NVB - 1)
                        if bp < NP2:
                            prefetch_u(gq + NUB - 1)
                        if bg is not None and bp < NP2:
                            want = ((bp + 1) * nsteps) // NP2
                            while done_steps < want:
                                next(bg, None)
                                done_steps += 1
                    if bg is not None:
                        for _ in bg:
                            pass
                    for j in range(NJ):
                        T = t0 + j
                        DMA(lambda e, T=T: e.dma_start(out=hbC[:], in_=h_scr[T * P:(T + 1) * P, :]),
                            r=["h_scr"], w=["hbC"])
                        for n in range(2):
                            ybk = YB[j][n]
                            DVE(lambda e, n=n, ybk=ybk: e.tensor_tensor(
                                out=hbC[:, n * 512:(n + 1) * 512], in0=banks[ybk][:], in1=hbC[:, n * 512:(n + 1) * 512],
                                op=ALU.add),
                                r=[("ps", ybk), "hbC"], w=["hbC"])
                        ACT(lambda e: e.activation(out=ob[:], in_=hbC[:], func=AF.Square, scale=1.0 / 32.0,
                                                   accum_out=ms3[:, 0:1]),
                            r=["hbC"], w=["ob", "ms3"])
                        ACT(lambda e: e.activation(out=ms3[:, 2:3], in_=ms3[:, 0:1], func=AF.Sqrt, bias=epsT[:, 0:1], scale=1.0),
                            r=["ms3", "epsT"], w=["ms3s"])
                        DVE(lambda e: e.reciprocal(out=ms3[:, 1:2], in_=ms3[:, 2:3]), r=["ms3s"], w=["ms3r"])
                        DVE(lambda e: e.scalar_tensor_tensor(out=ob[:], in0=hbC[:], scalar=ms3[:, 1:2], in1=fgB[:],
                                                             op0=ALU.mult, op1=ALU.mult),
                            r=["hbC", "ms3r", "fgB"], w=["ob"])
                        out_dmas.append(DMA(lambda e, T=T: e.dma_start(out=out[T * P:(T + 1) * P, :], in_=ob[:]),
                                            r=["ob"], w=["out"]))
            sch.barrier()

        sch.barrier()
        sch.final_wait(out_dmas)
        n_wait = sch.lower()
    return nc


def _vecT(v):
    return np.ascontiguousarray(np.asarray(v, np.float32).reshape(8, P).T)


def make_in_maps(inputs, n_cores=8, n_tok=S):
    f = lambda k: np.asarray(inputs[k], np.float32)
    consts = _const_tables()
    vecs = np.stack([_vecT(f("norm1_gain")[0]), _vecT(f("norm2_gain")[0]), _vecT(f("mem_norm_gain")[0]),
                     _vecT(f("pool_scale")[0]), _vecT(f("sgu_ln_gain")[0]), _vecT(f("sgu_ln_bias")[0])], axis=1)
    U = f("peer_u")[0]
    UTh = np.ascontiguousarray(U.reshape(P, P, 8, P).transpose(0, 3, 2, 1)).reshape(P * P, 8 * P)
    wq = f("peer_w_q")[0]
    wqT = np.ascontiguousarray(wq.reshape(D, 16, P).transpose(2, 1, 0))
    keysT = np.ascontiguousarray(np.stack([f("peer_keys1")[0].T, f("peer_keys2")[0].T], axis=1))
    shared = {
        "w_in": np.ascontiguousarray(f("w_in")[0]),
        "vecs": np.ascontiguousarray(vecs),
        "fg": np.ascontiguousarray(f("final_norm_gain").reshape(1, D)),
        "pool_w": np.ascontiguousarray(f("pool_w")[0]),
        "wsT": np.ascontiguousarray(f("sgu_w_s")[0].transpose(2, 0, 1)),
        "bs": np.ascontiguousarray(f("sgu_b_s")[0].reshape(1, 8 * P)),
        "swo": np.ascontiguousarray(f("sgu_w_out")[0]),
        "wkv": np.ascontiguousarray(f("xa_w_kv")[0]),
        "xwo": np.ascontiguousarray(f("xa_w_out")[0]),
        "wout": np.ascontiguousarray(f("w_out")[0]),
        "wqT": wqT,
        "keysT": keysT,
        "UTh": UTh,
        "Vh": np.ascontiguousarray(f("peer_v")[0]),
    }
    shared.update(consts)
    x = f("x")
    mem = f("mem")
    maps = []
    for b in range(n_cores):
        m = dict(shared)
        m["x"] = np.ascontiguousarray(x[b, :n_tok])
        m["mem"] = np.ascontiguousarray(mem[b])
        maps.append(m)
    return maps


def kernel(**inputs):
    nc = build()
    in_maps = make_in_maps(inputs)
    res = run_bass_kernel_spmd(nc, in_maps, core_ids=list(range(8)))
    return np.stack([np.asarray(r["out"], np.float32) for r in res.results], axis=0)
```
